# Optimizing a Trainium2 kernel written in Bass

```python
import jax, jax.numpy as jnp
from jax import lax
import numpy as np

D_MODEL = 1024
BATCH = 4
SEQ = 4096
DEPTH = 4

N_EVEN = (DEPTH + 1) // 2
N_ODD = DEPTH // 2
EPS = 1e-6
LRU_WIDTH = D_MODEL // 2
LRU_BLOCKS = 8
LRU_BLOCK = LRU_WIDTH // LRU_BLOCKS
CONV_W = 4
LRU_C = 8.0
FOX_HEADS = 8
FOX_HEAD_DIM = (D_MODEL // 2) // FOX_HEADS
FOX_WIDTH = FOX_HEADS * FOX_HEAD_DIM
Q_BLOCK = 128
EVEN_SPLITS = [LRU_WIDTH, 2 * LRU_WIDTH, 2 * LRU_WIDTH + FOX_WIDTH, 2 * LRU_WIDTH + 2 * FOX_WIDTH, 2 * LRU_WIDTH + 3 * FOX_WIDTH]
EVEN_IN = 2 * LRU_WIDTH + 3 * FOX_WIDTH + FOX_HEADS
GLA_HEADS = 4
GLA_DK = (D_MODEL // 2) // GLA_HEADS
GLA_DV = D_MODEL // GLA_HEADS
GLA_RANK = 16
GLA_TAU = 16.0
GLA_CHUNK = 64
GLA_KW = GLA_HEADS * GLA_DK
GLA_VW = GLA_HEADS * GLA_DV
ODD_SPLITS = [GLA_KW, 2 * GLA_KW, 2 * GLA_KW + GLA_VW, 2 * GLA_KW + 2 * GLA_VW]
ODD_IN = 2 * GLA_KW + 2 * GLA_VW + GLA_RANK
D_FF = 2816
N_EXPERTS = 8
TOP_K = 2
D_FF_EXPERT = 3584

kernel_name = "hybrid_rglru_fox_gla_moe"


def rms_norm(x, g):
    xf = x.astype(jnp.float32)
    y = xf * lax.rsqrt(jnp.mean(xf * xf, axis=-1, keepdims=True) + EPS)
    return (y * g.astype(jnp.float32)).astype(x.dtype)


def causal_depthwise_conv(x, w, b):
    s = x.shape[1]
    xp = jnp.pad(x, ((0, 0), (CONV_W - 1, 0), (0, 0)))
    out = b
    for j in range(CONV_W):
        out = out + xp[:, j:j + s] * w[j]
    return out


def block_diag_linear(x, w, b):
    xb = x.reshape(x.shape[0], x.shape[1], LRU_BLOCKS, LRU_BLOCK)
    return jnp.einsum('bsni,nij->bsnj', xb, w).reshape(x.shape) + b


def rglru(x, ga_w, ga_b, gx_w, gx_b, lam):
    xf = x.astype(jnp.float32)
    r = jax.nn.sigmoid(block_diag_linear(xf, ga_w.astype(jnp.float32), ga_b.astype(jnp.float32)))
    i = jax.nn.sigmoid(block_diag_linear(xf, gx_w.astype(jnp.float32), gx_b.astype(jnp.float32)))
    log_a = -LRU_C * r * jax.nn.softplus(-lam.astype(jnp.float32))
    a = jnp.exp(log_a)
    u = jnp.sqrt(-jnp.expm1(2.0 * log_a)) * (i * xf)

    def combine(c1, c2):
        a1, b1 = c1
        a2, b2 = c2
        return a1 * a2, a2 * b1 + b2

    _, h = lax.associative_scan(combine, (a, u), axis=1)
    return h


def fox_attention(q, k, v, log_f):
    bsz, s, h, dh = q.shape
    nb = s // Q_BLOCK
    c = jnp.cumsum(log_f, axis=1).transpose(0, 2, 1)
    qh = q.transpose(0, 2, 1, 3) * (dh ** -0.5)
    kh = k.transpose(0, 2, 1, 3)
    vh = v.transpose(0, 2, 1, 3)
    qb = qh.reshape(bsz, h, nb, Q_BLOCK, dh).transpose(2, 0, 1, 3, 4)
    cb = c.reshape(bsz, h, nb, Q_BLOCK).transpose(2, 0, 1, 3)
    pos_b = jnp.arange(s).reshape(nb, Q_BLOCK)
    k_pos = jnp.arange(s)

    def one_block(args):
        q_blk, c_blk, p_blk = args
        logits = jnp.einsum('bhqd,bhkd->bhqk', q_blk, kh).astype(jnp.float32)
        logits = logits + c_blk[..., None] - c[:, :, None, :]
        mask = p_blk[:, None] >= k_pos[None, :]
        logits = jnp.where(mask, logits, -jnp.inf)
        p = jax.nn.softmax(logits, axis=-1)
        return jnp.einsum('bhqk,bhkd->bhqd', p.astype(vh.dtype), vh)

    ob = lax.map(one_block, (qb, cb, pos_b))
    return ob.transpose(1, 0, 3, 2, 4).reshape(bsz, s, h * dh)


def gla_chunked(q, k, v, log_a):
    bsz, s, h, dk = q.shape
    dv = v.shape[-1]
    n = s // GLA_CHUNK

    def chunks(t):
        return t.reshape(bsz, n, GLA_CHUNK, h, t.shape[-1]).transpose(1, 0, 3, 2, 4)

    qc, kc, vc, gc = chunks(q), chunks(k), chunks(v), chunks(log_a)
    b = jnp.cumsum(gc, axis=3)
    b_last = b[:, :, :, -1:, :]
    q_dec = qc * jnp.exp(b)
    k_intra = kc * jnp.exp(-b)
    k_state = kc * jnp.exp(b_last - b)
    chunk_decay = jnp.exp(b_last[:, :, :, 0, :])
    causal = jnp.tril(jnp.ones((GLA_CHUNK, GLA_CHUNK), dtype=bool))
    scores = jnp.where(causal, jnp.einsum('nbhid,nbhjd->nbhij', q_dec, k_intra), 0.0)
    o_intra = jnp.einsum('nbhij,nbhje->nbhie', scores, vc)

    def step(state, xs):
        q_n, k_n, v_n, d_n = xs
        o_n = jnp.einsum('bhid,bhde->bhie', q_n, state)
        state = d_n[..., None] * state + jnp.einsum('bhjd,bhje->bhde', k_n, v_n)
        return state, o_n

    state0 = jnp.zeros((bsz, h, dk, dv), jnp.float32)
    _, o_inter = lax.scan(step, state0, (q_dec, k_state, vc, chunk_decay))
    o = o_intra + o_inter
    return o.transpose(1, 0, 3, 2, 4).reshape(bsz, s, h, dv)


def even_mixer(hn, w_in, conv_w, conv_b, ga_w, ga_b, gx_w, gx_b, lam, f_b, w_out):
    bsz, s, _ = hn.shape
    proj = hn @ w_in
    xr, yr, q, k, v, f_logit = jnp.split(proj, EVEN_SPLITS, axis=-1)
    xc = causal_depthwise_conv(xr, conv_w, conv_b)
    h_lru = rglru(xc, ga_w, ga_b, gx_w, gx_b, lam)
    y_a = (h_lru * jax.nn.gelu(yr.astype(jnp.float32))).astype(hn.dtype)
    log_f = jax.nn.log_sigmoid((f_logit + f_b).astype(jnp.float32))
    shp = (bsz, s, FOX_HEADS, FOX_HEAD_DIM)
    y_b = fox_attention(q.reshape(shp), k.reshape(shp), v.reshape(shp), log_f).astype(hn.dtype)
    return jnp.concatenate([y_a, y_b], axis=-1) @ w_out


def odd_mixer(hn, w_in, gate_w2, gate_b, head_norm, w_out):
    bsz, s, _ = hn.shape
    proj = hn @ w_in
    q, k, v, g, lr = jnp.split(proj, ODD_SPLITS, axis=-1)
    log_a = jax.nn.log_sigmoid((lr @ gate_w2 + gate_b).astype(jnp.float32)) / GLA_TAU
    kshp = (bsz, s, GLA_HEADS, GLA_DK)
    o = gla_chunked((q.astype(jnp.float32) * (GLA_DK ** -0.5)).reshape(kshp),
                    k.astype(jnp.float32).reshape(kshp),
                    v.astype(jnp.float32).reshape(bsz, s, GLA_HEADS, GLA_DV),
                    log_a.reshape(kshp))
    o = o * lax.rsqrt(jnp.mean(o * o, axis=-1, keepdims=True) + EPS)
    o = o * head_norm.astype(jnp.float32).reshape(GLA_HEADS, GLA_DV)
    o = o.reshape(bsz, s, GLA_VW) * jax.nn.silu(g.astype(jnp.float32))
    return o.astype(hn.dtype) @ w_out


def swiglu(hn, w1, w3, w2):
    return (jax.nn.silu(hn @ w1) * (hn @ w3)) @ w2


def moe_swiglu(hn, router, w1, w3, w2):
    bsz, s, d = hn.shape
    t = hn.reshape(-1, d)
    logits = (t @ router).astype(jnp.float32)
    top_vals, top_idx = lax.top_k(logits, TOP_K)
    top_w = jax.nn.softmax(top_vals, axis=-1)
    gates = jnp.sum(top_w[..., None] * jax.nn.one_hot(top_idx, N_EXPERTS, dtype=jnp.float32), axis=1)
    out = jnp.zeros_like(t)
    for e in range(N_EXPERTS):
        h_e = jax.nn.silu(t @ w1[e]) * (t @ w3[e])
        out = out + gates[:, e:e + 1].astype(t.dtype) * (h_e @ w2[e])
    return out.reshape(bsz, s, d)


def setup_inputs(seed: int = 0) -> dict:
    key = jax.random.key(seed)
    ks = iter(jax.random.split(key, 32))
    f32 = jnp.float32
    out_scale = (2.0 * DEPTH) ** -0.5

    def dense(shape, fan_in, scale=1.0):
        return jax.random.normal(next(ks), shape, f32) * (scale * fan_in ** -0.5)

    def gain(shape):
        return 1.0 + 0.02 * jax.random.normal(next(ks), shape, f32)

    def small(shape):
        return 0.02 * jax.random.normal(next(ks), shape, f32)

    x = jax.random.normal(next(ks), (BATCH, SEQ, D_MODEL), f32)
    norm_mix = gain((DEPTH, D_MODEL))
    norm_ffn = gain((DEPTH, D_MODEL))
    norm_final = gain((D_MODEL,))
    ev_w_in = dense((N_EVEN, D_MODEL, EVEN_IN), D_MODEL)
    ev_conv_w = dense((N_EVEN, CONV_W, LRU_WIDTH), CONV_W)
    ev_conv_b = small((N_EVEN, LRU_WIDTH))
    ev_ga_w = dense((N_EVEN, LRU_BLOCKS, LRU_BLOCK, LRU_BLOCK), LRU_BLOCK)
    ev_ga_b = small((N_EVEN, LRU_WIDTH))
    ev_gx_w = dense((N_EVEN, LRU_BLOCKS, LRU_BLOCK, LRU_BLOCK), LRU_BLOCK)
    ev_gx_b = small((N_EVEN, LRU_WIDTH))
    u = jax.random.uniform(next(ks), (N_EVEN, LRU_WIDTH), f32, minval=0.9, maxval=0.999)
    p = u ** (1.0 / LRU_C)
    ev_lambda = jnp.log(p) - jnp.log1p(-p)
    ev_f_b = 2.0 + 0.1 * jax.random.normal(next(ks), (N_EVEN, FOX_HEADS), f32)
    ev_w_out = dense((N_EVEN, D_MODEL, D_MODEL), D_MODEL, out_scale)
    ev_ffn_w1 = dense((N_EVEN, D_MODEL, D_FF), D_MODEL)
    ev_ffn_w3 = dense((N_EVEN, D_MODEL, D_FF), D_MODEL)
    ev_ffn_w2 = dense((N_EVEN, D_FF, D_MODEL), D_FF, out_scale)
    od_w_in = dense((N_ODD, D_MODEL, ODD_IN), D_MODEL)
    od_gate_w2 = dense((N_ODD, GLA_RANK, GLA_KW), GLA_RANK)
    od_gate_b = small((N_ODD, GLA_KW))
    od_head_norm = gain((N_ODD, GLA_VW))
    od_w_out = dense((N_ODD, GLA_VW, D_MODEL), GLA_VW, out_scale)
    od_router = dense((N_ODD, D_MODEL, N_EXPERTS), D_MODEL)
    od_exp_w1 = dense((N_ODD, N_EXPERTS, D_MODEL, D_FF_EXPERT), D_MODEL)
    od_exp_w3 = dense((N_ODD, N_EXPERTS, D_MODEL, D_FF_EXPERT), D_MODEL)
    od_exp_w2 = dense((N_ODD, N_EXPERTS, D_FF_EXPERT, D_MODEL), D_FF_EXPERT, out_scale)
    return {"x": x, "norm_mix": norm_mix, "norm_ffn": norm_ffn, "norm_final": norm_final,
            "ev_w_in": ev_w_in, "ev_conv_w": ev_conv_w, "ev_conv_b": ev_conv_b,
            "ev_ga_w": ev_ga_w, "ev_ga_b": ev_ga_b, "ev_gx_w": ev_gx_w, "ev_gx_b": ev_gx_b,
            "ev_lambda": ev_lambda, "ev_f_b": ev_f_b, "ev_w_out": ev_w_out,
            "ev_ffn_w1": ev_ffn_w1, "ev_ffn_w3": ev_ffn_w3, "ev_ffn_w2": ev_ffn_w2,
            "od_w_in": od_w_in, "od_gate_w2": od_gate_w2, "od_gate_b": od_gate_b,
            "od_head_norm": od_head_norm, "od_w_out": od_w_out, "od_router": od_router,
            "od_exp_w1": od_exp_w1, "od_exp_w3": od_exp_w3, "od_exp_w2": od_exp_w2}


def reference(x, norm_mix, norm_ffn, norm_final,
              ev_w_in, ev_conv_w, ev_conv_b, ev_ga_w, ev_ga_b, ev_gx_w, ev_gx_b,
              ev_lambda, ev_f_b, ev_w_out, ev_ffn_w1, ev_ffn_w3, ev_ffn_w2,
              od_w_in, od_gate_w2, od_gate_b, od_head_norm, od_w_out, od_router,
              od_exp_w1, od_exp_w3, od_exp_w2):
    h = x
    for layer in range(DEPTH):
        j = layer // 2
        hn = rms_norm(h, norm_mix[layer])
        if layer % 2 == 0:
            h = h + even_mixer(hn, ev_w_in[j], ev_conv_w[j], ev_conv_b[j], ev_ga_w[j], ev_ga_b[j],
                               ev_gx_w[j], ev_gx_b[j], ev_lambda[j], ev_f_b[j], ev_w_out[j])
            h = h + swiglu(rms_norm(h, norm_ffn[layer]), ev_ffn_w1[j], ev_ffn_w3[j], ev_ffn_w2[j])
        else:
            h = h + odd_mixer(hn, od_w_in[j], od_gate_w2[j], od_gate_b[j], od_head_norm[j], od_w_out[j])
            h = h + moe_swiglu(rms_norm(h, norm_ffn[layer]), od_router[j], od_exp_w1[j],
                               od_exp_w3[j], od_exp_w2[j])
    return rms_norm(h, norm_final)
```

```python
import numpy as np
import concourse.bass as bass
import concourse.mybir as mybir
from concourse.bass_utils import run_bass_kernel_spmd
from contextlib import ExitStack

F32 = mybir.dt.float32
BF16 = mybir.dt.bfloat16
AF = mybir.ActivationFunctionType
ALU = mybir.AluOpType
AX = mybir.AxisListType

ENGS = ("pe", "act", "dve", "pool", "sp")
EIDX = {e: i for i, e in enumerate(ENGS)}


class Buf:
    __slots__ = ("w", "r")

    def __init__(self):
        self.w = None
        self.r = []


class Op:
    __slots__ = ("eng", "fn", "is_dma", "deps", "k", "inc", "dsem", "dval", "clock", "waited", "cinc")

    def __init__(self, eng, fn, is_dma):
        self.eng = eng
        self.fn = fn
        self.is_dma = is_dma
        self.deps = []
        self.k = 0
        self.inc = 0
        self.dsem = None
        self.dval = 0
        self.clock = None
        self.waited = False
        self.cinc = 16


class Sched:
    def __init__(self, nc, stack, n_dma_sems=40):
        self.nc = nc
        self.eng = {"pe": nc.tensor, "act": nc.scalar, "dve": nc.vector,
                    "pool": nc.gpsimd, "sp": nc.sync}
        self.ops = []
        self.sems = {e: stack.enter_context(nc.semaphore("s_" + e)) for e in ENGS}
        self.dsems = [stack.enter_context(nc.semaphore("d%d" % i)) for i in range(n_dma_sems)]
        self.cnt = {e: 0 for e in ENGS}
        self.last_clock = {e: [0] * len(ENGS) for e in ENGS}
        self.dma_known = {e: set() for e in ENGS}
        self.inc_count = {e: 0 for e in ENGS}
        nd = n_dma_sems
        self.dcount = [0] * nd
        self.dprev = [None] * nd
        self.di = 0
        self.total = 0

    def op(self, eng, fn, reads=(), writes=(), dma=False):
        o = Op(eng, fn, dma)
        deps = set()
        for b in reads:
            if b.w is not None:
                deps.add(b.w)
        for b in writes:
            if b.w is not None:
                deps.add(b.w)
            for r in b.r:
                deps.add(r)
        o.deps = list(deps)
        for b in reads:
            b.r.append(o)
        for b in writes:
            b.w = o
            b.r = []
        self.ops.append(o)
        return o

    def dma(self, fn, reads=(), writes=(), eng="sp"):
        return self.op(eng, fn, reads, writes, dma=True)

    def emit(self, barrier=True):
        ops = self.ops
        self.ops = []
        self.total += len(ops)
        nE = len(ENGS)
        for o in ops:
            self.cnt[o.eng] += 1
            o.k = self.cnt[o.eng]
        needed = []
        for o in ops:
            clk = list(self.last_clock[o.eng])
            known = self.dma_known[o.eng]
            waits = []
            for d in sorted(o.deps, key=lambda d: -d.k):
                if d.is_dma:
                    if d.k == 0 or d in known:
                        continue
                    known.add(d)
                    waits.append(d)
                    d.waited = True
                    dc = d.clock
                    for i in range(nE):
                        if dc[i] > clk[i]:
                            clk[i] = dc[i]
                else:
                    j = EIDX[d.eng]
                    if d.eng == o.eng and o.eng == "pe":
                        continue
                    if clk[j] >= d.k:
                        continue
                    waits.append(d)
                    d.waited = True
                    dc = d.clock
                    for i in range(nE):
                        if dc[i] > clk[i]:
                            clk[i] = dc[i]
                    if clk[j] < d.k:
                        clk[j] = d.k
            self.last_clock[o.eng] = clk
            oc = list(clk)
            if not o.is_dma:
                oc[EIDX[o.eng]] = o.k
            o.clock = oc
            needed.append(waits)
        last_of = {}
        if barrier:
            for o in ops:
                if not o.is_dma:
                    last_of[o.eng] = o
            for o in last_of.values():
                o.waited = True
        nd = len(self.dsems)
        for o in ops:
            if o.is_dma:
                s = self.di % nd
                self.di += 1
                o.dsem = s
                self.dcount[s] += o.cinc
                o.dval = self.dcount[s]
            elif o.waited:
                self.inc_count[o.eng] += 1
                o.inc = self.inc_count[o.eng]
        for o, waits in zip(ops, needed):
            e = self.eng[o.eng]
            if o.is_dma and self.dprev[o.dsem] is not None:
                p = self.dprev[o.dsem]
                e.wait_ge(self.dsems[p.dsem], p.dval)
            for d in waits:
                if d.is_dma:
                    e.wait_ge(self.dsems[d.dsem], d.dval)
                else:
                    e.wait_ge(self.sems[d.eng], d.inc)
            ins = o.fn()
            if o.is_dma:
                ins.then_inc(self.dsems[o.dsem], o.cinc)
                self.dprev[o.dsem] = o
            elif o.waited:
                ins.then_inc(self.sems[o.eng], 1)
        if barrier:
            for en in ENGS:
                e = self.eng[en]
                for e2 in ENGS:
                    if self.inc_count[e2] > 0 and e2 != "sp":
                        e.wait_ge(self.sems[e2], self.inc_count[e2])
                for p in self.dprev:
                    if p is not None:
                        e.wait_ge(self.dsems[p.dsem], p.dval)
                self.last_clock[en] = [self.cnt[x] for x in ENGS]
            for o in ops:
                o.k = 0


class Ctx:
    def __init__(self):
        self.nc = bass.Bass("TRN2", target_bir_lowering=False)
        self.st = ExitStack()
        self.S = Sched(self.nc, self.st)
        self.n = 0
        self.E = self.S.eng

    def din(self, name, shape, dt=F32):
        return self.nc.dram_tensor(name, list(shape), dt, kind="ExternalInput").ap()

    def dout(self, name, shape, dt=F32):
        return self.nc.dram_tensor(name, list(shape), dt, kind="ExternalOutput").ap()

    def sb(self, shape, dt, st=None):
        self.n += 1
        return (st or self.st).enter_context(self.nc.sbuf_tensor("t%d" % self.n, list(shape), dt))

    def ps(self, shape=(128, 512), dt=F32, st=None):
        self.n += 1
        return (st or self.st).enter_context(self.nc.psum_tensor("p%d" % self.n, list(shape), dt))

    def op(self, eng, method, reads, writes, *a, **kw):
        e = self.E[eng]
        return self.S.op(eng, lambda: getattr(e, method)(*a, **kw), reads, writes)

    def dma(self, out, in_, reads, writes, eng="sp"):
        e = self.E[eng]
        return self.S.dma(lambda: e.dma_start(out=out, in_=in_), reads, writes, eng=eng)

    def close(self):
        self.S.emit(barrier=True)
        self.st.close()
        return self.nc


class Rot:
    def __init__(self, items):
        self.items = items
        self.i = 0

    def next(self):
        it = self.items[self.i % len(self.items)]
        self.i += 1
        return it


S_LEN = 4096
NB = S_LEN // 512
EPS = 1e-6


def load_consts(C, names=("ident", "e127", "tri", "bdtri", "sel")):
    out = {}
    if not hasattr(C, "cdram"):
        C.cdram = {}
    for nm in names:
        shape = [8, 1024] if nm == "sel" else [128, 128]
        if nm not in C.cdram:
            C.cdram[nm] = C.din("c_" + nm, shape)
        d = C.cdram[nm]
        t = C.sb(shape, F32)
        b = Buf()
        C.dma(t[:], d, [], [b])
        out[nm] = (t, b)
    return out


def make_misc(C):
    ones_bf = C.sb([128, 128], BF16); b1 = Buf()
    C.op("pool", "memset", [], [b1], ones_bf[:], 1.0)
    cols = C.sb([128, 4], F32); b2 = Buf()
    C.op("pool", "memset", [], [b2], cols[:, 0:1], EPS)
    C.op("pool", "memset", [], [b2], cols[:, 1:2], 1.0)
    C.op("pool", "memset", [], [b2], cols[:, 2:3], 0.0)
    ones_f = C.sb([128, 512], F32); b3 = Buf()
    C.op("pool", "memset", [], [b3], ones_f[:], 1.0)
    return dict(ones_bf=(ones_bf, b1), cols=(cols, b2), ones_f=(ones_f, b3))


def load_w_bf16(C, dst, dst_buf, src_view, ncols, stage, stage_buf, eng_cycle=("dve", "pool"), step=512):
    i = 0
    for c0 in range(0, ncols, step):
        n = min(step, ncols - c0)
        C.dma(stage[:, :, 0:n], src_view[:, :, c0:c0 + n], [], [stage_buf])
        eng = eng_cycle[i % len(eng_cycle)]
        i += 1
        C.op(eng, "tensor_copy", [stage_buf], [dst_buf], out=dst[:, :, c0:c0 + n], in_=stage[:, :, 0:n])


def norm_block(C, M, hb, hb_buf, g, g_buf, hn, hn_buf, sq, sq_buf, rstd, rstd_buf, pst, ps_buf, n=512, nch=8,
               hn32=None, hn32_buf=None, inv_d=1.0 / 1024):
    ones_bf, ob = M["ones_bf"]
    cols, cb = M["cols"]
    C.op("act", "activation", [hb_buf], [sq_buf], out=sq[:, 0:nch, 0:n], in_=hb[:, 0:nch, 0:n], func=AF.Square)
    for c in range(nch):
        C.op("pe", "matmul", [ob, sq_buf], [ps_buf], pst[:, 0:n], lhsT=ones_bf[:], rhs=sq[:, c, 0:n],
             start=(c == 0), stop=(c == nch - 1))
    C.op("act", "activation", [ps_buf, cb], [rstd_buf], out=rstd[:, 0:n], in_=pst[:, 0:n], func=AF.Ln,
         bias=cols[:, 0:1], scale=inv_d)
    C.op("act", "activation", [rstd_buf], [rstd_buf], out=rstd[:, 0:n], in_=rstd[:, 0:n], func=AF.Exp, scale=-0.5)
    for c in range(nch):
        eng = "dve"
        C.op(eng, "scalar_tensor_tensor", [hb_buf, g_buf, rstd_buf], [hn_buf], out=hn[:, c, 0:n], in0=hb[:, c, 0:n],
             scalar=g[:, c:c + 1], in1=rstd[:, 0:n], op0=ALU.mult, op1=ALU.mult)
        if hn32 is not None:
            eng2 = "dve"
            C.op(eng2, "scalar_tensor_tensor", [hb_buf, g_buf, rstd_buf], [hn32_buf], out=hn32[:, c, 0:n],
                 in0=hb[:, c, 0:n], scalar=g[:, c:c + 1], in1=rstd[:, 0:n], op0=ALU.mult, op1=ALU.mult)


def build_mix_even(phases=(1, 2, 3)):
    C = Ctx()
    hT = C.din("hT", [1024, S_LEN]).rearrange("(c p) t -> p c t", p=128)
    yT = C.dout("yT", [512, S_LEN], BF16)
    mix_even_body(C, "", lambda blk: [(hT[:, :, blk * 512:(blk + 1) * 512], 0, 8)], lambda r0, r1: yT[r0:r1], [], [], phases)
    return C.close()


def mix_even_body(C, pre, hblk, yrow, h_rd, y_wr, phases=(1, 2, 3)):
    old_st = C.st
    C.st = ExitStack()
    nc = C.nc
    gmix = C.din(pre + "gmix", [128, 8])
    w = C.din(pre + "w", [1024, 1284]).rearrange("(c p) n -> p c n", p=128)
    convw = C.din(pre + "convw", [128, 2, 4])
    vecs = C.din(pre + "vecs", [128, 2, 4])
    gaw = C.din(pre + "gaw", [4, 64, 64])
    gxw = C.din(pre + "gxw", [4, 64, 64])
    fb = C.din(pre + "fb", [4, 1])

    K = load_consts(C, ("ident", "e127", "tri"))
    M = make_misc(C)
    cols, cb = M["cols"]

    Wb = C.sb([128, 8, 1284], BF16); Wb_b = Buf()
    g_t = C.sb([128, 8], F32); g_b = Buf()
    C.dma(g_t[:], gmix, [], [g_b])
    cw_t = C.sb([128, 2, 4], F32); cw_b = Buf()
    C.dma(cw_t[:], convw, [], [cw_b])
    vc_t = C.sb([128, 2, 4], F32); vc_b = Buf()
    C.dma(vc_t[:], vecs, [], [vc_b])
    fb_t = C.sb([4, 1], F32); fb_b = Buf()
    C.dma(fb_t[:], fb, [], [fb_b])

    qT = C.sb([128, 2, S_LEN], BF16); q_b = [[Buf() for _ in range(NB)] for _ in range(2)]
    kT = C.sb([128, 2, S_LEN], BF16); k_b = [[Buf() for _ in range(NB)] for _ in range(2)]
    vx = C.sb([128, 32, 4, 65], BF16); v_b = [Buf() for _ in range(32)]
    fT = C.sb([4, S_LEN], F32); f_b = Buf()

    pxy = ExitStack()
    xrT = C.sb([128, 2, S_LEN], F32, pxy); xr_b = [[Buf() for _ in range(NB)] for _ in range(2)]
    yrT = C.sb([128, 2, S_LEN], F32, pxy); yr_b = [[Buf() for _ in range(NB)] for _ in range(2)]
    C.op("pool", "memset", [], v_b, vx[:, :, :, 64:65], 1.0)

    with ExitStack() as p1:
        hb = [C.sb([128, 8, 512], F32, p1) for _ in range(2)]; hb_b = [Buf(), Buf()]
        load_w_bf16(C, Wb, Wb_b, w, 1284, hb[1], hb_b[1])
        sq = C.sb([128, 8, 512], BF16, p1); sq_b = Buf()
        rstd = C.sb([128, 512], F32, p1); rstd_b = Buf()
        hn = C.sb([128, 8, 512], BF16, p1); hn_b = Buf()
        pss = Rot([(C.ps(st=p1), Buf()) for _ in range(8)])
        for blk in range(NB):
            t0 = blk * 512
            h_t, h_b = hb[blk % 2], hb_b[blk % 2]
            for (src_, lo_, hi_) in hblk(blk):
                C.dma(h_t[:, lo_:hi_, :], src_, h_rd, [h_b])
            pst, psb = pss.next()
            norm_block(C, M, h_t, h_b, g_t, g_b, hn, hn_b, sq, sq_b, rstd, rstd_b, pst, psb)
            for oc in range(8):
                pst, psb = pss.next()
                for kc in range(8):
                    C.op("pe", "matmul", [Wb_b, hn_b], [psb], pst[:], lhsT=Wb[:, kc, oc * 128:(oc + 1) * 128],
                         rhs=hn[:, kc, :], start=(kc == 0), stop=(kc == 7))
                if oc < 2:
                    C.op("act", "copy", [psb], [xr_b[oc][blk]], out=xrT[:, oc, t0:t0 + 512], in_=pst[:])
                elif oc < 4:
                    C.op("dve", "tensor_copy", [psb], [yr_b[oc - 2][blk]], out=yrT[:, oc - 2, t0:t0 + 512], in_=pst[:])
                elif oc < 6:
                    C.op("act", "copy", [psb], [q_b[oc - 4][blk]], out=qT[:, oc - 4, t0:t0 + 512], in_=pst[:])
                else:
                    C.op("dve", "tensor_copy", [psb], [k_b[oc - 6][blk]], out=kT[:, oc - 6, t0:t0 + 512], in_=pst[:])
            pst, psb = pss.next()
            for kc in range(8):
                C.op("pe", "matmul", [Wb_b, hn_b], [psb], pst[0:4, :], lhsT=Wb[:, kc, 1280:1284],
                     rhs=hn[:, kc, :], start=(kc == 0), stop=(kc == 7))
            C.op("act", "copy", [psb], [f_b], out=fT[:, t0:t0 + 512], in_=pst[0:4, :])
            for sub in range(4):
                pst, psb = pss.next()
                for kc in range(8):
                    C.op("pe", "matmul", [Wb_b, hn_b], [psb], pst[:, 0:256], lhsT=hn[:, kc, sub * 128:(sub + 1) * 128],
                         rhs=Wb[:, kc, 1024:1280], start=(kc == 0), stop=(kc == 7))
                tb = blk * 4 + sub
                C.op("dve" if sub % 2 else "act", "tensor_copy" if sub % 2 else "copy", [psb], [v_b[tb]],
                     out=vx[:, tb, :, 0:64], in_=pst[:, 0:256].rearrange("p (h d) -> p h d", h=4))
        C.S.emit(barrier=True)

    with ExitStack() as p2:
      if 2 in phases:
          TB = 1024
          wst = C.sb([128, 2, 2, 128], F32, p2); wst_b = Buf()
          C.op("pool", "memset", [], [wst_b], wst[:], 0.0)
          for c in range(2):
              for n in range(2):
                  blkid = c * 2 + n
                  C.dma(wst[n * 64:(n + 1) * 64, c, 0, n * 64:(n + 1) * 64], gaw[blkid], [], [wst_b])
                  C.dma(wst[n * 64:(n + 1) * 64, c, 1, n * 64:(n + 1) * 64], gxw[blkid], [], [wst_b])
          wbd = C.sb([128, 2, 2, 128], BF16, p2); wbd_b = Buf()
          C.op("dve", "tensor_copy", [wst_b], [wbd_b], out=wbd[:], in_=wst[:])
          sc = C.sb([128, 2, 4], F32, p2); sc_b = Buf()
          C.op("act", "activation", [vc_b], [sc_b], out=sc[:, :, 0], in_=vc_t[:, :, 3], func=AF.Exp, scale=-1.0)
          C.op("act", "activation", [sc_b, cb], [sc_b], out=sc[:, :, 1], in_=sc[:, :, 0], func=AF.Ln, bias=cols[:, 1:2])
          C.op("dve", "tensor_scalar", [sc_b], [sc_b], out=sc[:, :, 2], in0=sc[:, :, 1], scalar1=-8.0, scalar2=None, op0=ALU.mult)
          C.op("dve", "tensor_scalar", [sc_b], [sc_b], out=sc[:, :, 3], in0=sc[:, :, 1], scalar1=-16.0, scalar2=None, op0=ALU.mult)

          def T(dt=F32):
              return C.sb([128, TB], dt, p2), Buf()
          xc, xc_b = T(); xcb, xcb_b = T(BF16); r_t, r_b = T(); i_t, i_b = T(); a_t, a_b = T(); s_t, s_b = T()
          u_t, u_b = T(); g1, g1_b = T(); g2, g2_b = T(); yo, yo_b = T(BF16)
          hsc = [C.sb([128, TB], F32, p2) for _ in range(2)]; hsc_b = [Buf(), Buf()]
          pss = Rot([(C.ps(st=p2), Buf()) for _ in range(4)])
          for c in range(2):
              allx = [xr_b[c][b] for b in range(NB)]
              ally = [yr_b[c][b] for b in range(NB)]
              for tb in range(S_LEN // TB):
                  t0 = tb * TB
                  C.op("dve", "tensor_scalar", allx + [cw_b, vc_b], [xc_b], out=xc[:], in0=xrT[:, c, t0:t0 + TB],
                       scalar1=cw_t[:, c, 3:4], scalar2=vc_t[:, c, 0:1], op0=ALU.mult, op1=ALU.add)
                  for d in (1, 2, 3):
                      o0 = d if t0 == 0 else 0
                      eng = "dve"
                      C.op(eng, "scalar_tensor_tensor", allx + [cw_b, xc_b], [xc_b], out=xc[:, o0:TB],
                           in0=xrT[:, c, t0 + o0 - d:t0 + TB - d], scalar=cw_t[:, c, 3 - d:4 - d], in1=xc[:, o0:TB],
                           op0=ALU.mult, op1=ALU.add)
                  C.op("act", "copy", [xc_b], [xcb_b], out=xcb[:], in_=xc[:])
                  for half in range(TB // 512):
                      sl = slice(half * 512, (half + 1) * 512)
                      pa, pab = pss.next()
                      C.op("pe", "matmul", [wbd_b, xcb_b], [pab], pa[:], lhsT=wbd[:, c, 0, :], rhs=xcb[:, sl], start=True, stop=True)
                      C.op("act", "activation", [pab, vc_b], [r_b], out=r_t[:, sl], in_=pa[:], func=AF.Sigmoid, bias=vc_t[:, c, 1:2])
                      px, pxb = pss.next()
                      C.op("pe", "matmul", [wbd_b, xcb_b], [pxb], px[:], lhsT=wbd[:, c, 1, :], rhs=xcb[:, sl], start=True, stop=True)
                      C.op("act", "activation", [pxb, vc_b], [i_b], out=i_t[:, sl], in_=px[:], func=AF.Sigmoid, bias=vc_t[:, c, 2:3])
                  C.op("act", "activation", [r_b, sc_b], [a_b], out=a_t[:], in_=r_t[:], func=AF.Exp, scale=sc[:, c, 2:3])
                  C.op("act", "activation", [r_b, sc_b], [s_b], out=s_t[:], in_=r_t[:], func=AF.Exp, scale=sc[:, c, 3:4])
                  C.op("act", "activation", [s_b, cb], [s_b], out=s_t[:], in_=s_t[:], func=AF.Sqrt, scale=-1.0, bias=cols[:, 1:2])
                  C.op("pool", "tensor_tensor", [i_b, xc_b], [u_b], out=u_t[:], in0=i_t[:], in1=xc[:], op=ALU.mult)
                  C.op("dve", "tensor_tensor", [u_b, s_b], [u_b], out=u_t[:], in0=u_t[:], in1=s_t[:], op=ALU.mult)
                  hcur, hcur_b = hsc[tb % 2], hsc_b[tb % 2]
                  hprev, hprev_b = hsc[(tb + 1) % 2], hsc_b[(tb + 1) % 2]
                  if tb == 0:
                      C.op("dve", "tensor_tensor_scan", [a_b, u_b], [hcur_b], out=hcur[:], data0=a_t[:], data1=u_t[:],
                           initial=0.0, op0=ALU.mult, op1=ALU.add)
                  else:
                      C.op("dve", "tensor_tensor_scan", [a_b, u_b, hprev_b], [hcur_b], out=hcur[:], data0=a_t[:], data1=u_t[:],
                           initial=hprev[:, TB - 1:TB], op0=ALU.mult, op1=ALU.add)
                  yv = yrT[:, c, t0:t0 + TB]
                  C.op("act", "activation", ally, [g1_b], out=g1[:], in_=yv, func=AF.Square)
                  C.op("pool", "tensor_scalar", [g1_b], [g1_b], out=g1[:], in0=g1[:], scalar1=0.044715, scalar2=1.0,
                       op0=ALU.mult, op1=ALU.add)
                  C.op("pool", "tensor_tensor", ally + [g1_b], [g1_b], out=g1[:], in0=g1[:], in1=yv, op=ALU.mult)
                  C.op("act", "activation", [g1_b], [g2_b], out=g2[:], in_=g1[:], func=AF.Sigmoid, scale=1.5957691216057308)
                  C.op("pool", "tensor_tensor", ally + [g2_b], [g2_b], out=g2[:], in0=g2[:], in1=yv, op=ALU.mult)
                  C.op("dve", "tensor_tensor", [g2_b, hcur_b], [yo_b], out=yo[:], in0=g2[:], in1=hcur[:], op=ALU.mult)
                  C.dma(yrow(c * 128, (c + 1) * 128)[:, t0:t0 + TB], yo[:], [yo_b], y_wr)
          C.S.emit(barrier=True)

    pxy.close()
    with ExitStack() as p3:
      if 3 in phases:
          ident, id_b = K["ident"]; e127, e_b = K["e127"]; tri, tri_b = K["tri"]
          ones_f, of_b = M["ones_f"]
          tri_bf = C.sb([128, 128], BF16, p3); trb_b = Buf()
          C.op("dve", "tensor_copy", [tri_b], [trb_b], out=tri_bf[:], in_=tri[:])
          nfb = C.sb([4, 1], F32, p3); nfb_b = Buf()
          C.op("dve", "tensor_scalar", [fb_b], [nfb_b], out=nfb[:], in0=fb_t[:], scalar1=-1.0, scalar2=None, op0=ALU.mult)
          sp = C.sb([4, S_LEN], F32, p3); sp_b = Buf()
          C.op("act", "activation", [f_b, nfb_b], [sp_b], out=sp[:], in_=fT[:], func=AF.Exp, scale=-1.0, bias=nfb[:, 0:1])
          C.op("act", "activation", [sp_b, cb], [sp_b], out=sp[:], in_=sp[:], func=AF.Ln, bias=cols[0:4, 1:2])
          cpos = C.sb([4, S_LEN], F32, p3); cpos_b = Buf()
          for j in range(8):
              sl = slice(j * 512, (j + 1) * 512)
              init = 0.0 if j == 0 else cpos[:, j * 512 - 1:j * 512]
              C.op("dve", "tensor_tensor_scan", [sp_b, of_b, cpos_b], [cpos_b], out=cpos[:, sl], data0=ones_f[0:4, :],
                   data1=sp[:, sl], initial=init, op0=ALU.mult, op1=ALU.add)
          pcc = C.ps(st=p3); pcc_b = Buf()
          for j in range(32):
              C.op("pe", "matmul", [cpos_b, id_b], [pcc_b], pcc[:, j * 4:(j + 1) * 4], lhsT=cpos[:, j * 128:(j + 1) * 128],
                   rhs=ident[0:4, 0:4], start=True, stop=True)
          ccol = C.sb([128, 32, 4], F32, p3); ccol_b = Buf()
          C.op("dve", "tensor_copy", [pcc_b], [ccol_b], out=ccol[:], in_=pcc[:, 0:128].rearrange("p (j h) -> p j h", h=4))
          pbc = C.ps(st=p3); pbc_b = Buf()
          C.op("pe", "matmul", [ccol_b, e_b], [pbc_b], pbc[:, 0:128], lhsT=e127[:], rhs=ccol[:].rearrange("p j h -> p (j h)"),
               start=True, stop=True)
          bcv = C.sb([128, 32, 4], F32, p3); bcv_b = Buf()
          C.op("dve", "tensor_copy", [pbc_b], [bcv_b], out=bcv[:], in_=pbc[:, 0:128].rearrange("p (j h) -> p j h", h=4))
          tab = C.sb([128, 4, 32, 32], F32, p3); tab_b = Buf()
          for h in range(4):
              for j in range(32):
                  eng = "dve" if j % 2 == 0 else "pool"
                  C.op(eng, "tensor_scalar", [bcv_b, ccol_b], [tab_b], out=tab[:, h, j, :], in0=bcv[:, :, h],
                       scalar1=ccol[:, j, h:h + 1], scalar2=-1.0, op0=ALU.subtract, op1=ALU.mult)
          pS = Rot([(C.ps(st=p3), Buf()) for _ in range(3)])
          pO = Rot([(C.ps(st=p3), Buf()) for _ in range(2)])
          pB = C.ps(st=p3); pB_b = Buf()
          PT = Rot([(C.sb([128, 512], BF16, p3), Buf()) for _ in range(3)])
          osb = Rot([(C.sb([128, 512], F32, p3), Buf()) for _ in range(2)])
          rden = C.sb([128, 512], F32, p3); rden_b = Buf()
          yb = Rot([(C.sb([128, 512], BF16, p3), Buf()) for _ in range(2)])
          allq = lambda p_: [q_b[p_][b] for b in range(NB)]
          for h in range(4):
              p_ = h // 2
              pb = (h % 2) * 64
              for qb in range(8):
                  O, O_b = pO.next()
                  nk = 4 * qb + 4
                  for kb in range(nk):
                      i0 = max(kb - 4 * qb, 0)
                      qs = 512 * qb + 128 * i0
                      n = 512 - 128 * i0
                      ST, ST_b = pS.next()
                      C.op("pe", "matmul", [k_b[p_][kb // 4], q_b[p_][qb]], [ST_b], ST[:, 0:n],
                           lhsT=kT[pb:pb + 64, p_, kb * 128:(kb + 1) * 128], rhs=qT[pb:pb + 64, p_, qs:qs + n],
                           start=True, stop=True)
                      P, P_b = PT.next()
                      for i in range(i0, 4):
                          C.op("act", "activation", [ST_b, tab_b], [P_b], out=P[:, i * 128:(i + 1) * 128],
                               in_=ST[:, (i - i0) * 128:(i - i0 + 1) * 128], func=AF.Exp, scale=0.125,
                               bias=tab[:, h, kb, 4 * qb + i:4 * qb + i + 1])
                      if kb >= 4 * qb:
                          C.op("pool", "tensor_tensor", [P_b, trb_b], [P_b], out=P[:, i0 * 128:(i0 + 1) * 128],
                               in0=P[:, i0 * 128:(i0 + 1) * 128], in1=tri_bf[:], op=ALU.mult)
                      C.op("pe", "matmul", [v_b[kb], P_b], [O_b], O[0:65, 128 * i0:512], lhsT=vx[:, kb, h, :],
                           rhs=P[:, 128 * i0:512], start=(kb == 0), stop=(kb == nk - 1))
                  C.op("dve", "reciprocal", [O_b], [rden_b], out=rden[64:65, :], in_=O[64:65, :])
                  C.op("pe", "matmul", [rden_b, of_b], [pB_b], pB[0:64, :], lhsT=ones_f[64:65, 0:64], rhs=rden[64:65, :],
                       start=True, stop=True)
                  o_s, o_sb = osb.next()
                  C.op("act", "copy", [O_b], [o_sb], out=o_s[0:64, :], in_=O[0:64, :])
                  y_t, y_tb = yb.next()
                  C.op("dve", "tensor_tensor", [o_sb, pB_b], [y_tb], out=y_t[0:64, :], in0=o_s[0:64, :], in1=pB[0:64, :], op=ALU.mult)
                  C.dma(yrow(256 + h * 64, 256 + (h + 1) * 64)[:, qb * 512:(qb + 1) * 512], y_t[0:64, :], [y_tb], y_wr)
          C.S.emit(barrier=True)
    C.S.emit(barrier=True)
    C.st.close()
    C.st = old_st


def mix_even_inputs(inp, j, layer, b, g, hT_full, pre=""):
    W = inp["ev_w_in"][j]
    cs = lambda base, n: W[:, base + g * n: base + (g + 1) * n]
    w = np.concatenate([cs(0, 256), cs(512, 256), cs(1024, 256), cs(1536, 256), cs(2048, 256),
                        W[:, 2560 + g * 4:2560 + (g + 1) * 4]], axis=1)
    ch = slice(g * 256, (g + 1) * 256)
    pc = lambda v: np.ascontiguousarray(v.reshape(2, 128).T)
    convw = np.ascontiguousarray(inp["ev_conv_w"][j][:, ch].reshape(4, 2, 128).transpose(2, 1, 0))
    vecs = np.stack([pc(inp["ev_conv_b"][j][ch]), pc(inp["ev_ga_b"][j][ch]), pc(inp["ev_gx_b"][j][ch]),
                     pc(inp["ev_lambda"][j][ch])], axis=2)
    d = {
        "gmix": np.ascontiguousarray(inp["norm_mix"][layer].reshape(8, 128).T),
        "w": np.ascontiguousarray(w),
        "convw": convw, "vecs": np.ascontiguousarray(vecs),
        "gaw": np.ascontiguousarray(inp["ev_ga_w"][j][g * 4:(g + 1) * 4]),
        "gxw": np.ascontiguousarray(inp["ev_gx_w"][j][g * 4:(g + 1) * 4]),
        "fb": np.ascontiguousarray(inp["ev_f_b"][j][g * 4:(g + 1) * 4].reshape(4, 1)),
    }
    d = {pre + k: v for k, v in d.items()}
    if hT_full is not None:
        d["hT"] = np.ascontiguousarray(hT_full)
    return d


def const_inputs():
    ident = np.eye(128, dtype=np.float32)
    e127 = np.zeros((128, 128), np.float32); e127[127, :] = 1.0
    k = np.arange(128)
    tri = (k[:, None] <= k[None, :]).astype(np.float32)
    bdtri = tri * ((k[:, None] // 64) == (k[None, :] // 64))
    sel = np.zeros((8, 8, 128), np.float32)
    for e in range(8):
        sel[e, e, :] = 1.0
    return {"c_ident": ident, "c_e127": e127, "c_tri": tri, "c_bdtri": bdtri.astype(np.float32),
            "c_sel": sel.reshape(8, 1024)}


NT = 2048
NBC = NT // 512


def build_chan(kind, final):
    C = Ctx()
    hT = C.din("hT", [1024, NT]).rearrange("(c p) t -> p c t", p=128)
    yT = C.din("yT", [1024, NT], BF16).rearrange("(c p) t -> p c t", p=128)
    outT = C.dout("outT", [1024, NT]).rearrange("(c p) t -> p c t", p=128)
    chan_body(C, "", kind, final, [(hT, 0, 8)], [], yT, None, None, [], [(outT, 0, 8)], [])
    return C.close()


def chan_body(C, pre, kind, final, hT, h_rd, yT, y_all, rsel_d, y_rd, outT, out_wr):
    old_st = C.st
    C.st = ExitStack()
    wout = C.din(pre + "wout", [1024, 1024]).rearrange("(c p) n -> p c n", p=128)
    gffn = C.din(pre + "gffn", [128, 8])
    if kind == "ffn":
        DFF, NE = 2816, 1
        w1 = [C.din(pre + "w1", [1024, DFF]).rearrange("(c p) n -> p c n", p=128)]
        w3 = [C.din(pre + "w3", [1024, DFF]).rearrange("(c p) n -> p c n", p=128)]
        w2 = [C.din(pre + "w2", [DFF, 1024]).rearrange("(c p) n -> p c n", p=128)]
    else:
        DFF, NE = 3584, 8
        router = C.din(pre + "router", [1024, 8]).rearrange("(c p) n -> p c n", p=128)
        ew1 = C.din(pre + "ew1", [8, 1024, DFF]); ew3 = C.din(pre + "ew3", [8, 1024, DFF]); ew2 = C.din(pre + "ew2", [8, DFF, 1024])
        w1 = [ew1[e].rearrange("(c p) n -> p c n", p=128) for e in range(8)]
        w3 = [ew3[e].rearrange("(c p) n -> p c n", p=128) for e in range(8)]
        w2 = [ew2[e].rearrange("(c p) n -> p c n", p=128) for e in range(8)]
    if final:
        gfin = C.din(pre + "gfin", [128, 8])
    NFG = DFF // 256

    K = load_consts(C, ("ident", "sel") if kind == "moe" else ())
    M = make_misc(C)
    cols, cb = M["cols"]

    h = C.sb([128, 8, NT], F32); h_b = [[Buf() for _ in range(NBC)] for _ in range(8)]
    g_t = C.sb([128, 8], F32); g_b = Buf()
    C.dma(g_t[:], gffn, [], [g_b])
    if final:
        gf_t = C.sb([128, 8], F32); gf_b = Buf()
        C.dma(gf_t[:], gfin, [], [gf_b])
    for blk in range(NBC):
        for (src_, lo_, hi_) in hT:
            C.dma(h[:, lo_:hi_, blk * 512:(blk + 1) * 512], src_[:, :, blk * 512:(blk + 1) * 512], h_rd, [h_b[c][blk] for c in range(lo_, hi_)])

    with ExitStack() as p1:
        y = C.sb([128, 8, NT], BF16, p1); y_b = [Buf() for _ in range(NBC)]
        if y_all is None:
            for blk in range(NBC):
                C.dma(y[:, :, blk * 512:(blk + 1) * 512], yT[:, :, blk * 512:(blk + 1) * 512], y_rd, [y_b[blk]])
        else:
            rs_t = C.sb([128, 2], F32, p1); rs_b = Buf()
            C.dma(rs_t[:], rsel_d, [], [rs_b])
            yA = Rot([(C.sb([128, 8, 512], BF16, p1), Buf()) for _ in range(2)])
            yB = Rot([(C.sb([128, 8, 512], BF16, p1), Buf()) for _ in range(2)])
            for blk in range(NBC):
                sl = slice(blk * 512, (blk + 1) * 512)
                a_t, a_b = yA.next()
                b_t, b_b = yB.next()
                for qi, yv in enumerate(y_all):
                    C.dma(a_t[:, 4 * qi:4 * qi + 4, :], yv[:, :, blk * 512:(blk + 1) * 512], y_rd, [a_b])
                    C.dma(b_t[:, 4 * qi:4 * qi + 4, :], yv[:, :, NT + blk * 512:NT + (blk + 1) * 512], y_rd, [b_b])
                C.op("dve", "tensor_scalar", [a_b, rs_b], [a_b], out=a_t[:], in0=a_t[:], scalar1=rs_t[:, 0:1], scalar2=None, op0=ALU.mult)
                C.op("dve", "scalar_tensor_tensor", [a_b, b_b, rs_b], [y_b[blk]], out=y[:, :, sl], in0=b_t[:], scalar=rs_t[:, 1:2],
                     in1=a_t[:], op0=ALU.mult, op1=ALU.add)
        wo = C.sb([128, 8, 1024], BF16, p1); wo_b = Buf()
        stage = C.sb([128, 8, 512], F32, p1); stage_b = Buf()
        load_w_bf16(C, wo, wo_b, wout, 1024, stage, stage_b)
        pss = Rot([(C.ps(st=p1), Buf()) for _ in range(4)])
        for blk in range(NBC):
            sl = slice(blk * 512, (blk + 1) * 512)
            for oc in range(8):
                pst, psb = pss.next()
                for kc in range(8):
                    C.op("pe", "matmul", [wo_b, y_b[blk]], [psb], pst[:], lhsT=wo[:, kc, oc * 128:(oc + 1) * 128],
                         rhs=y[:, kc, sl], start=(kc == 0), stop=(kc == 7))
                C.op("dve", "tensor_tensor", [psb, h_b[oc][blk]], [h_b[oc][blk]], out=h[:, oc, sl], in0=h[:, oc, sl],
                     in1=pst[:], op=ALU.add)
        C.S.emit(barrier=True)

    hn = C.sb([128, 8, NT], BF16); hn_b = [Buf() for _ in range(NBC)]
    if kind == "moe":
        gTs = C.sb([8, NT], F32); gTs_b = Buf()
    with ExitStack() as p2:
        sq = C.sb([128, 8, 512], BF16, p2); sq_b = Buf()
        rstd = C.sb([128, 512], F32, p2); rstd_b = Buf()
        pn = C.ps(st=p2); pn_b = Buf()
        if kind == "moe":
            hn32 = C.sb([128, 8, 512], F32, p2); hn32_b = Buf()
            r32 = C.sb([128, 8, 8], F32, p2); r32_b = Buf()
            C.dma(r32[:], router, [], [r32_b])
            ident, id_b = K["ident"]
            plg = Rot([(C.ps(st=p2), Buf()) for _ in range(2)])
            pgt = Rot([(C.ps(st=p2), Buf()) for _ in range(2)])
            sm = Rot([(C.sb([128, 64], F32, p2), Buf()) for _ in range(2)])
        for blk in range(NBC):
            sl = slice(blk * 512, (blk + 1) * 512)
            allh = [h_b[c][blk] for c in range(8)]
            hb = h[:, :, sl]
            ones_bf, ob = M["ones_bf"]
            C.op("act", "activation", allh, [sq_b], out=sq[:], in_=hb, func=AF.Square)
            for c in range(8):
                C.op("pe", "matmul", [ob, sq_b], [pn_b], pn[:], lhsT=ones_bf[:], rhs=sq[:, c, :], start=(c == 0), stop=(c == 7))
            C.op("act", "activation", [pn_b, cb], [rstd_b], out=rstd[:], in_=pn[:], func=AF.Ln, bias=cols[:, 0:1], scale=1.0 / 1024)
            C.op("act", "activation", [rstd_b], [rstd_b], out=rstd[:], in_=rstd[:], func=AF.Exp, scale=-0.5)
            for c in range(8):
                C.op("dve", "scalar_tensor_tensor", [h_b[c][blk], g_b, rstd_b], [hn_b[blk]], out=hn[:, c, sl], in0=h[:, c, sl],
                     scalar=g_t[:, c:c + 1], in1=rstd[:], op0=ALU.mult, op1=ALU.mult)
                if kind == "moe":
                    C.op("dve", "scalar_tensor_tensor", [h_b[c][blk], g_b, rstd_b], [hn32_b], out=hn32[:, c, :], in0=h[:, c, sl],
                         scalar=g_t[:, c:c + 1], in1=rstd[:], op0=ALU.mult, op1=ALU.mult)
            if kind == "moe":
                for sub in range(4):
                    lg, lg_b = plg.next()
                    for kc in range(8):
                        C.op("pe", "matmul", [hn32_b, r32_b], [lg_b], lg[:, 0:8], lhsT=hn32[:, kc, sub * 128:(sub + 1) * 128],
                             rhs=r32[:, kc, :], start=(kc == 0), stop=(kc == 7))
                    s, s_b = sm.next()
                    C.op("dve", "tensor_copy", [lg_b], [s_b], out=s[:, 0:8], in_=lg[:, 0:8])
                    C.op("dve", "reduce_max", [s_b], [s_b], out=s[:, 40:41], in_=s[:, 0:8], axis=AX.X)
                    C.op("dve", "tensor_scalar", [s_b], [s_b], out=s[:, 8:16], in0=s[:, 0:8], scalar1=s[:, 40:41], scalar2=None,
                         op0=ALU.is_equal)
                    C.op("dve", "scalar_tensor_tensor", [s_b], [s_b], out=s[:, 16:24], in0=s[:, 8:16], scalar=-1e30, in1=s[:, 0:8],
                         op0=ALU.mult, op1=ALU.add)
                    C.op("dve", "reduce_max", [s_b], [s_b], out=s[:, 41:42], in_=s[:, 16:24], axis=AX.X)
                    C.op("dve", "tensor_scalar", [s_b], [s_b], out=s[:, 24:32], in0=s[:, 16:24], scalar1=s[:, 41:42], scalar2=None,
                         op0=ALU.is_equal)
                    C.op("dve", "tensor_tensor", [s_b], [s_b], out=s[:, 42:43], in0=s[:, 41:42], in1=s[:, 40:41], op=ALU.subtract)
                    C.op("act", "activation", [s_b], [s_b], out=s[:, 44:45], in_=s[:, 42:43], func=AF.Sigmoid)
                    C.op("act", "activation", [s_b], [s_b], out=s[:, 43:44], in_=s[:, 42:43], func=AF.Sigmoid, scale=-1.0)
                    C.op("dve", "tensor_scalar", [s_b], [s_b], out=s[:, 32:40], in0=s[:, 8:16], scalar1=s[:, 43:44], scalar2=None,
                         op0=ALU.mult)
                    C.op("dve", "scalar_tensor_tensor", [s_b], [s_b], out=s[:, 32:40], in0=s[:, 24:32], scalar=s[:, 44:45],
                         in1=s[:, 32:40], op0=ALU.mult, op1=ALU.add)
                    gt, gt_b = pgt.next()
                    C.op("pe", "matmul", [s_b, id_b], [gt_b], gt[0:8, 0:128], lhsT=s[:, 32:40], rhs=ident[:], start=True, stop=True)
                    c0 = blk * 512 + sub * 128
                    C.op("act", "copy", [gt_b], [gTs_b], out=gTs[:, c0:c0 + 128], in_=gt[0:8, 0:128])
        C.S.emit(barrier=True)

    with ExitStack() as p3:
        st1 = [C.sb([128, 8, 256], F32, p3) for _ in range(2)]
        st3 = [C.sb([128, 8, 256], F32, p3) for _ in range(2)]
        st2 = [C.sb([128, 2, 1024], F32, p3) for _ in range(2)]
        st_b = [[Buf(), Buf(), Buf()] for _ in range(2)]
        wb1 = [C.sb([128, 8, 256], BF16, p3) for _ in range(2)]
        wb3 = [C.sb([128, 8, 256], BF16, p3) for _ in range(2)]
        wb2 = [C.sb([128, 2, 1024], BF16, p3) for _ in range(2)]
        wb_b = [[Buf(), Buf(), Buf()] for _ in range(2)]
        pA = Rot([(C.ps(st=p3), Buf()) for _ in range(2)])
        pB = Rot([(C.ps(st=p3), Buf()) for _ in range(2)])
        pO = Rot([(C.ps(st=p3), Buf()) for _ in range(3)])
        pG = C.ps(st=p3); pG_b = Buf()
        slu = Rot([(C.sb([128, 512], F32, p3), Buf()) for _ in range(2)])
        hid = Rot([(C.sb([128, 2, 512], BF16, p3), Buf()) for _ in range(2)])
        if kind == "moe":
            sel, sel_b = K["sel"]
            Gt = [C.sb([128, NBC, 512], BF16, p3) for _ in range(2)]; Gt_b = [Buf(), Buf()]
        it = 0
        for e in range(NE):
            if kind == "moe":
                G, G_b = Gt[e % 2], Gt_b[e % 2]
                for blk in range(NBC):
                    C.op("pe", "matmul", [sel_b, gTs_b], [pG_b], pG[:], lhsT=sel[0:8, e * 128:(e + 1) * 128],
                         rhs=gTs[0:8, blk * 512:(blk + 1) * 512], start=True, stop=True)
                    C.op("act", "copy", [pG_b], [G_b], out=G[:, blk, :], in_=pG[:])
            for fg in range(NFG):
                par = it % 2
                it += 1
                f0 = fg * 256
                C.dma(st1[par][:], w1[e][:, :, f0:f0 + 256], [], [st_b[par][0]])
                C.dma(st3[par][:], w3[e][:, :, f0:f0 + 256], [], [st_b[par][1]])
                C.dma(st2[par][:], w2[e][:, 2 * fg:2 * fg + 2, :], [], [st_b[par][2]])
                C.op("pool", "tensor_copy", [st_b[par][0]], [wb_b[par][0]], out=wb1[par][:], in_=st1[par][:])
                C.op("pool", "tensor_copy", [st_b[par][1]], [wb_b[par][1]], out=wb3[par][:], in_=st3[par][:])
                C.op("pool", "tensor_copy", [st_b[par][2]], [wb_b[par][2]], out=wb2[par][:], in_=st2[par][:])
                for blk in range(NBC):
                    sl = slice(blk * 512, (blk + 1) * 512)
                    hd, hd_b = hid.next()
                    for fc in range(2):
                        a, a_b = pA.next()
                        for kc in range(8):
                            C.op("pe", "matmul", [wb_b[par][0], hn_b[blk]], [a_b], a[:], lhsT=wb1[par][:, kc, fc * 128:(fc + 1) * 128],
                                 rhs=hn[:, kc, sl], start=(kc == 0), stop=(kc == 7))
                        b3, b3_b = pB.next()
                        for kc in range(8):
                            C.op("pe", "matmul", [wb_b[par][1], hn_b[blk]], [b3_b], b3[:], lhsT=wb3[par][:, kc, fc * 128:(fc + 1) * 128],
                                 rhs=hn[:, kc, sl], start=(kc == 0), stop=(kc == 7))
                        s_t, s_tb = slu.next()
                        C.op("act", "activation", [a_b], [s_tb], out=s_t[:], in_=a[:], func=AF.Silu)
                        C.op("dve", "tensor_tensor", [s_tb, b3_b], [hd_b], out=hd[:, fc, :], in0=s_t[:], in1=b3[:], op=ALU.mult)
                        if kind == "moe":
                            C.op("pool", "tensor_tensor", [hd_b, G_b], [hd_b], out=hd[:, fc, :], in0=hd[:, fc, :], in1=G[:, blk, :],
                                 op=ALU.mult)
                    for dc in range(8):
                        o, o_b = pO.next()
                        for fc in range(2):
                            C.op("pe", "matmul", [wb_b[par][2], hd_b], [o_b], o[:], lhsT=wb2[par][:, fc, dc * 128:(dc + 1) * 128],
                                 rhs=hd[:, fc, :], start=(fc == 0), stop=(fc == 1))
                        C.op("dve", "tensor_tensor", [o_b, h_b[dc][blk]], [h_b[dc][blk]], out=h[:, dc, sl], in0=h[:, dc, sl],
                             in1=o[:], op=ALU.add)
        C.S.emit(barrier=True)

    with ExitStack() as p4:
        if final:
            sq = C.sb([128, 8, 512], BF16, p4); sq_b = Buf()
            rstd = C.sb([128, 512], F32, p4); rstd_b = Buf()
            pn = C.ps(st=p4); pn_b = Buf()
            ob32 = Rot([(C.sb([128, 8, 512], F32, p4), Buf()) for _ in range(2)])
            ones_bf, ob = M["ones_bf"]
        for blk in range(NBC):
            sl = slice(blk * 512, (blk + 1) * 512)
            allh = [h_b[c][blk] for c in range(8)]
            if final:
                C.op("act", "activation", allh, [sq_b], out=sq[:], in_=h[:, :, sl], func=AF.Square)
                for c in range(8):
                    C.op("pe", "matmul", [ob, sq_b], [pn_b], pn[:], lhsT=ones_bf[:], rhs=sq[:, c, :], start=(c == 0), stop=(c == 7))
                C.op("act", "activation", [pn_b, cb], [rstd_b], out=rstd[:], in_=pn[:], func=AF.Ln, bias=cols[:, 0:1], scale=1.0 / 1024)
                C.op("act", "activation", [rstd_b], [rstd_b], out=rstd[:], in_=rstd[:], func=AF.Exp, scale=-0.5)
                o32, o32_b = ob32.next()
                for c in range(8):
                    C.op("dve", "scalar_tensor_tensor", allh + [gf_b, rstd_b], [o32_b], out=o32[:, c, :], in0=h[:, c, sl],
                         scalar=gf_t[:, c:c + 1], in1=rstd[:], op0=ALU.mult, op1=ALU.mult)
                for (dst_, lo_, hi_) in outT:
                    C.dma(dst_[:, :, sl], o32[:, lo_:hi_, :], [o32_b], out_wr)
            else:
                for (dst_, lo_, hi_) in outT:
                    C.dma(dst_[:, :, sl], h[:, lo_:hi_, sl], allh, out_wr)
    C.S.emit(barrier=True)
    C.st.close()
    C.st = old_st


def chan_inputs(inp, layer, hT_tok, yT_tok, final, pre="", perm=False):
    j = layer // 2
    d = {"gffn": np.ascontiguousarray(inp["norm_ffn"][layer].reshape(8, 128).T)}
    if layer % 2 == 0:
        d.update(wout=inp["ev_w_out"][j], w1=inp["ev_ffn_w1"][j], w3=inp["ev_ffn_w3"][j], w2=inp["ev_ffn_w2"][j])
    else:
        wo = inp["od_w_out"][j]
        if perm:
            wo = np.concatenate([wo[0:256], wo[512:768], wo[256:512], wo[768:1024]], axis=0)
        d.update(wout=np.ascontiguousarray(wo), router=inp["od_router"][j], ew1=inp["od_exp_w1"][j], ew3=inp["od_exp_w3"][j],
                 ew2=inp["od_exp_w2"][j])
    if final:
        d["gfin"] = np.ascontiguousarray(inp["norm_final"].reshape(8, 128).T)
    d = {pre + k: v for k, v in d.items()}
    if hT_tok is not None:
        d["hT"] = np.ascontiguousarray(hT_tok)
        d["yT"] = np.ascontiguousarray(yT_tok)
        if layer % 2 == 1:
            cm = const_inputs()
            d.update(c_ident=cm["c_ident"], c_sel=cm["c_sel"])
    return d


def build_mix_odd():
    C = Ctx()
    hT = C.din("hT", [1024, S_LEN]).rearrange("(c p) t -> p c t", p=128)
    yT = C.dout("yT", [512, S_LEN], BF16)
    mix_odd_body(C, "", lambda blk: [(hT[:, :, blk * 512:(blk + 1) * 512], 0, 8)], lambda r0, r1: yT[r0:r1], [], [])
    return C.close()


def mix_odd_body(C, pre, hblk, yrow, h_rd, y_wr):
    old_st = C.st
    C.st = ExitStack()
    gmix = C.din(pre + "gmix", [128, 8])
    w = C.din(pre + "w", [1024, 1552]).rearrange("(c p) n -> p c n", p=128)
    gw2 = C.din(pre + "gw2", [16, 256])
    gvec = C.din(pre + "gvec", [128, 2])
    hnorm = C.din(pre + "hnorm", [128, 4])

    K = load_consts(C, ("ident", "bdtri"))
    M = make_misc(C)
    cols, cb = M["cols"]
    ones_bf, ob = M["ones_bf"]
    ident, id_b = K["ident"]
    bdtri, bd_b = K["bdtri"]

    g_t = C.sb([128, 8], F32); g_b = Buf()
    C.dma(g_t[:], gmix, [], [g_b])
    gv_t = C.sb([128, 2], F32); gv_b = Buf()
    C.dma(gv_t[:], gvec, [], [gv_b])
    hnm = C.sb([128, 4], F32); hnm_b = Buf()
    C.dma(hnm[:], hnorm, [], [hnm_b])
    ngv = C.sb([128, 2], F32); ngv_b = Buf()
    C.op("dve", "tensor_scalar", [gv_b], [ngv_b], out=ngv[:], in0=gv_t[:], scalar1=-1.0, scalar2=None, op0=ALU.mult)
    id_bf = C.sb([128, 128], BF16); idb_b = Buf()
    C.op("dve", "tensor_copy", [id_b], [idb_b], out=id_bf[:], in_=ident[:])

    qd = C.sb([128, 2, S_LEN], BF16); qd_b = [Buf() for _ in range(NB)]
    kiT = C.sb([128, 2, S_LEN], BF16); ki_b = [Buf() for _ in range(NB)]
    kitok = C.sb([128, 32, 2, 128], BF16); kt_b = [Buf() for _ in range(32)]
    vtok = C.sb([128, 32, 512], BF16); vt_b = [Buf() for _ in range(32)]
    gS = C.sb([128, 4, S_LEN], BF16); gs_b = [Buf() for _ in range(NB)]
    dec = C.sb([128, 2, 64], F32); dec_b = Buf()

    with ExitStack() as p1:
        Wb = C.sb([128, 8, 1552], BF16, p1); Wb_b = Buf()
        hb = [C.sb([128, 8, 512], F32, p1) for _ in range(2)]; hb_b = [Buf(), Buf()]
        load_w_bf16(C, Wb, Wb_b, w, 1552, hb[1], hb_b[1])
        gw2f = C.sb([16, 256], F32, p1); gw2f_b = Buf()
        C.dma(gw2f[:], gw2, [], [gw2f_b])
        gw2b = C.sb([16, 256], BF16, p1); gw2b_b = Buf()
        C.op("dve", "tensor_copy", [gw2f_b], [gw2b_b], out=gw2b[:], in_=gw2f[:])
        mask01 = C.sb([128, 512], F32, p1); mk_b = Buf()
        C.op("pool", "memset", [], [mk_b], mask01[:], 1.0)
        C.op("pool", "memset", [mk_b], [mk_b], mask01[:].rearrange("p (c j) -> p c j", j=64)[:, :, 0:1], 0.0)
        sq = C.sb([128, 8, 512], BF16, p1); sq_b = Buf()
        rstd = C.sb([128, 512], F32, p1); rstd_b = Buf()
        hn = C.sb([128, 8, 512], BF16, p1); hn_b = Buf()
        lrb = C.sb([16, 512], BF16, p1); lrb_b = Buf()
        tE = Rot([(C.sb([128, 512], F32, p1), Buf()) for _ in range(1)])
        tB = Rot([(C.sb([128, 512], F32, p1), Buf()) for _ in range(1)])
        tQ = Rot([(C.sb([128, 512], F32, p1), Buf()) for _ in range(2)])
        tK = Rot([(C.sb([128, 512], F32, p1), Buf()) for _ in range(1)])
        pss = Rot([(C.ps(st=p1), Buf()) for _ in range(8)])
        for blk in range(NB):
            t0 = blk * 512
            sl = slice(t0, t0 + 512)
            h_t, h_b = hb[blk % 2], hb_b[blk % 2]
            for (src_, lo_, hi_) in hblk(blk):
                C.dma(h_t[:, lo_:hi_, :], src_, h_rd, [h_b])
            pst, psb = pss.next()
            norm_block(C, M, h_t, h_b, g_t, g_b, hn, hn_b, sq, sq_b, rstd, rstd_b, pst, psb)

            def proj(c0, n, pst, psb):
                for kc in range(8):
                    C.op("pe", "matmul", [Wb_b, hn_b], [psb], pst[0:n, :], lhsT=Wb[:, kc, c0:c0 + n], rhs=hn[:, kc, :],
                         start=(kc == 0), stop=(kc == 7))
            pst, psb = pss.next()
            proj(1536, 16, pst, psb)
            C.op("act", "copy", [psb], [lrb_b], out=lrb[:], in_=pst[0:16, :])
            for c in range(4):
                pst, psb = pss.next()
                proj(1024 + c * 128, 128, pst, psb)
                C.op("act", "activation", [psb], [gs_b[blk]], out=gS[:, c, sl], in_=pst[:], func=AF.Silu)
            for hl in range(2):
                pl, pl_b = pss.next()
                C.op("pe", "matmul", [gw2b_b, lrb_b], [pl_b], pl[:], lhsT=gw2b[:, hl * 128:(hl + 1) * 128], rhs=lrb[:],
                     start=True, stop=True)
                e_t, e_b = tE.next()
                C.op("act", "activation", [pl_b, ngv_b], [e_b], out=e_t[:], in_=pl[:], func=AF.Exp, scale=-1.0, bias=ngv[:, hl:hl + 1])
                C.op("act", "activation", [e_b, cb], [e_b], out=e_t[:], in_=e_t[:], func=AF.Ln, bias=cols[:, 1:2])
                B_t, B_b = tB.next()
                C.op("dve", "tensor_tensor_scan", [mk_b, e_b], [B_b], out=B_t[:], data0=mask01[:], data1=e_t[:], initial=0.0,
                     op0=ALU.mult, op1=ALU.add)
                q_t, q_b_ = tQ.next()
                k_t, k_b_ = tK.next()
                C.op("act", "activation", [B_b], [q_b_], out=q_t[:], in_=B_t[:], func=AF.Exp, scale=-1.0 / 16)
                C.op("act", "activation", [B_b], [k_b_], out=k_t[:], in_=B_t[:], func=AF.Exp, scale=1.0 / 16)
                C.op("pool", "tensor_copy", [q_b_], [dec_b], out=dec[:, hl, blk * 8:(blk + 1) * 8],
                     in_=q_t[:].rearrange("p (c j) -> p c j", j=64)[:, :, 63])
                pq, pq_b = pss.next()
                proj(hl * 128, 128, pq, pq_b)
                C.op("dve", "scalar_tensor_tensor", [pq_b, q_b_], [qd_b[blk]], out=qd[:, hl, sl], in0=pq[:], scalar=128 ** -0.5,
                     in1=q_t[:], op0=ALU.mult, op1=ALU.mult)
                pk, pk_b = pss.next()
                proj(256 + hl * 128, 128, pk, pk_b)
                C.op("dve", "tensor_tensor", [pk_b, k_b_], [ki_b[blk]], out=kiT[:, hl, sl], in0=pk[:], in1=k_t[:], op=ALU.mult)
                pt, pt_b = pss.next()
                for sub in range(4):
                    C.op("pe", "matmul", [ki_b[blk], idb_b], [pt_b], pt[:, sub * 128:(sub + 1) * 128],
                         lhsT=kiT[:, hl, t0 + sub * 128:t0 + (sub + 1) * 128], rhs=id_bf[:], start=True, stop=True)
                C.op("act", "copy", [pt_b], [kt_b[blk * 4 + s_] for s_ in range(4)], out=kitok[:, blk * 4:(blk + 1) * 4, hl, :],
                     in_=pt[:].rearrange("p (s d) -> p s d", d=128))
            for sub in range(4):
                pv, pv_b = pss.next()
                for kc in range(8):
                    C.op("pe", "matmul", [Wb_b, hn_b], [pv_b], pv[:], lhsT=hn[:, kc, sub * 128:(sub + 1) * 128],
                         rhs=Wb[:, kc, 512:1024], start=(kc == 0), stop=(kc == 7))
                tb = blk * 4 + sub
                C.op("dve" if sub % 2 else "act", "tensor_copy" if sub % 2 else "copy", [pv_b], [vt_b[tb]],
                     out=vtok[:, tb, :], in_=pv[:])
        C.S.emit(barrier=True)

    with ExitStack() as p2:
        S32 = [C.sb([128, 256], F32, p2) for _ in range(2)]; S32_b = [Buf(), Buf()]
        Sbf = [[C.sb([128, 256], BF16, p2) for _ in range(2)] for _ in range(2)]; Sbf_b = [[Buf(), Buf()] for _ in range(2)]
        Tt = [C.sb([128, 256], F32, p2) for _ in range(2)]; Tt_b = [Buf(), Buf()]
        for hl in range(2):
            C.op("pool", "memset", [], [S32_b[hl]], S32[hl][:], 0.0)
            C.op("pool", "memset", [], [Sbf_b[hl][0]], Sbf[hl][0][:], 0.0)
        pSc = Rot([(C.ps(st=p2), Buf()) for _ in range(2)])
        pOo = [[(C.ps(st=p2), Buf()) for _ in range(2)] for _ in range(2)]
        pP = (C.ps(st=p2), Buf())
        pN = (C.ps(st=p2), Buf())
        scm = Rot([(C.sb([128, 128], BF16, p2), Buf()) for _ in range(3)])
        sq2 = C.sb([128, 2, 512], BF16, p2); sq2_b = Buf()
        rs2 = C.sb([128, 512], F32, p2); rs2_b = Buf()
        ytmp = Rot([(C.sb([128, 512], F32, p2), Buf()) for _ in range(2)])
        yo = Rot([(C.sb([128, 512], BF16, p2), Buf()) for _ in range(2)])
        cur = [0, 0]
        for tb in range(32):
            blk = tb // 4
            t0 = tb * 128
            col = (tb % 4) * 128
            for hl in range(2):
                sc, sc_b = pSc.next()
                C.op("pe", "matmul", [ki_b[blk], qd_b[blk]], [sc_b], sc[:, 0:128], lhsT=kiT[:, hl, t0:t0 + 128],
                     rhs=qd[:, hl, t0:t0 + 128], start=True, stop=True)
                sm_t, sm_b = scm.next()
                C.op("dve", "tensor_tensor", [sc_b, bd_b], [sm_b], out=sm_t[:], in0=sc[:, 0:128], in1=bdtri[:], op=ALU.mult)
                for eh in range(2):
                    o, o_b = pOo[hl][eh]
                    C.op("pe", "matmul", [vt_b[tb], sm_b], [o_b], o[:, col:col + 128],
                         lhsT=vtok[:, tb, hl * 256 + eh * 128:hl * 256 + (eh + 1) * 128], rhs=sm_t[:], start=True, stop=False)
                for half in range(2):
                    n = tb * 2 + half
                    pb = 64 * half
                    c_ = cur[hl]
                    sb_t, sb_b = Sbf[hl][c_], Sbf_b[hl][c_]
                    for eh in range(2):
                        o, o_b = pOo[hl][eh]
                        C.op("pe", "matmul", [sb_b, qd_b[blk]], [o_b], o[:, col + pb:col + pb + 64],
                             lhsT=sb_t[:, eh * 128:(eh + 1) * 128], rhs=qd[:, hl, t0 + pb:t0 + pb + 64], start=False, stop=(half == 1))
                    P, P_b = pP
                    C.op("pe", "matmul", [kt_b[tb], vt_b[tb]], [P_b], P[:, 0:256], lhsT=kitok[pb:pb + 64, tb, hl, :],
                         rhs=vtok[pb:pb + 64, tb, hl * 256:(hl + 1) * 256], start=True, stop=True)
                    C.op("dve", "tensor_tensor", [P_b, S32_b[hl]], [Tt_b[hl]], out=Tt[hl][:], in0=S32[hl][:], in1=P[:, 0:256], op=ALU.add)
                    nx = 1 - c_
                    C.op("act", "mul", [Tt_b[hl], dec_b], [S32_b[hl]], out=S32[hl][:], in_=Tt[hl][:], mul=dec[:, hl, n:n + 1])
                    C.op("pool", "tensor_scalar", [Tt_b[hl], dec_b], [Sbf_b[hl][nx]], out=Sbf[hl][nx][:], in0=Tt[hl][:],
                         scalar1=dec[:, hl, n:n + 1], scalar2=None, op0=ALU.mult)
                    cur[hl] = nx
                if tb % 4 == 3:
                    sl = slice(blk * 512, (blk + 1) * 512)
                    for eh in range(2):
                        o, o_b = pOo[hl][eh]
                        C.op("act", "activation", [o_b], [sq2_b], out=sq2[:, eh, :], in_=o[:], func=AF.Square)
                    pn, pn_b = pN
                    for eh in range(2):
                        C.op("pe", "matmul", [ob, sq2_b], [pn_b], pn[:], lhsT=ones_bf[:], rhs=sq2[:, eh, :], start=(eh == 0), stop=(eh == 1))
                    C.op("act", "activation", [pn_b, cb], [rs2_b], out=rs2[:], in_=pn[:], func=AF.Ln, bias=cols[:, 0:1], scale=1.0 / 256)
                    C.op("act", "activation", [rs2_b], [rs2_b], out=rs2[:], in_=rs2[:], func=AF.Exp, scale=-0.5)
                    for eh in range(2):
                        o, o_b = pOo[hl][eh]
                        c = hl * 2 + eh
                        yt, yt_b = ytmp.next()
                        C.op("dve", "scalar_tensor_tensor", [o_b, hnm_b, rs2_b], [yt_b], out=yt[:], in0=o[:], scalar=hnm[:, c:c + 1],
                             in1=rs2[:], op0=ALU.mult, op1=ALU.mult)
                        y_t, y_b = yo.next()
                        C.op("pool", "tensor_tensor", [yt_b, gs_b[blk]], [y_b], out=y_t[:], in0=yt[:], in1=gS[:, c, sl], op=ALU.mult)
                        C.dma(yrow(c * 128, (c + 1) * 128)[:, sl], y_t[:], [y_b], y_wr)
        C.S.emit(barrier=True)
    C.S.emit(barrier=True)
    C.st.close()
    C.st = old_st


def mix_odd_inputs(inp, j, layer, b, g, hT_full, pre=""):
    W = inp["od_w_in"][j]
    w = np.concatenate([W[:, g * 256:(g + 1) * 256], W[:, 512 + g * 256:512 + (g + 1) * 256],
                        W[:, 1024 + g * 512:1024 + (g + 1) * 512], W[:, 2048 + g * 512:2048 + (g + 1) * 512],
                        W[:, 3072:3088]], axis=1)
    d = {
        "gmix": np.ascontiguousarray(inp["norm_mix"][layer].reshape(8, 128).T),
        "w": np.ascontiguousarray(w),
        "gw2": np.ascontiguousarray(inp["od_gate_w2"][j][:, g * 256:(g + 1) * 256]),
        "gvec": np.ascontiguousarray(inp["od_gate_b"][j][g * 256:(g + 1) * 256].reshape(2, 128).T),
        "hnorm": np.ascontiguousarray(inp["od_head_norm"][j][g * 512:(g + 1) * 512].reshape(4, 128).T),
    }
    d = {pre + k: v for k, v in d.items()}
    if hT_full is not None:
        d["hT"] = np.ascontiguousarray(hT_full)
        cm = const_inputs()
        d.update(c_ident=cm["c_ident"], c_bdtri=cm["c_bdtri"])
    return d


PAIRS = [[0, 1], [2, 3], [4, 5], [6, 7]]


def _gather(C, src, src_b, dst, dst_b):
    e = C.nc.gpsimd
    o = C.S.dma(lambda a=src, g=dst: e.collective_compute("AllGather", ALU.bypass, replica_groups=PAIRS, ins=[a], outs=[g]),
                [src_b], [dst_b], eng="pool")
    o.cinc = 1
    return o


def build_fused(upto=8):
    C = Ctx()
    nc = C.nc
    x_full = C.din("x_full", [1024, 4096]).rearrange("(c p) t -> p c t", p=128)
    x_own = C.din("x_own", [1024, 2048]).rearrange("(c p) t -> p c t", p=128)
    rsel = C.din("rsel", [128, 2])
    outT = C.dout("outT", [1024, 2048]).rearrange("(c p) t -> p c t", p=128)
    hprev, hprev_b = [(x_own, 0, 8)], []
    hx_all, hx_all_b = None, None
    stage = 0
    for L in range(4):
        pre = "L%d_" % L
        even = (L % 2 == 0)
        final = (L == 3)
        y_own = [nc.dram_tensor("y_own%d_%d" % (L, q), [256, 4096], BF16, kind="Internal").ap() for q in range(2)]
        y_own_b = [Buf(), Buf()]
        yrow = lambda r0, r1, y_own=y_own: y_own[r0 // 256][r0 % 256:(r1 - 1) % 256 + 1]
        if L == 0:
            hblk = lambda blk: [(x_full[:, :, blk * 512:(blk + 1) * 512], 0, 8)]
            h_rd = []
        else:
            hv = [[hx_all[q][r * 256:(r + 1) * 256, :].rearrange("(c p) t -> p c t", p=128) for q in range(4)] for r in range(2)]
            hblk = lambda blk, hv=hv: [(hv[blk // 4][q][:, :, (blk % 4) * 512:(blk % 4 + 1) * 512], 2 * q, 2 * q + 2) for q in range(4)]
            h_rd = hx_all_b
        if even:
            mix_even_body(C, pre, hblk, yrow, h_rd, y_own_b)
        else:
            mix_odd_body(C, pre, hblk, yrow, h_rd, y_own_b)
        stage += 1
        if stage >= upto:
            dbg = C.dout("dbg_y", [512, 4096], BF16)
            for q in range(2):
                C.dma(dbg[q * 256:(q + 1) * 256], y_own[q], y_own_b, [])
            break
        y_all = [nc.dram_tensor("y_all%d_%d" % (L, q), [512, 4096], BF16, kind="Internal").ap() for q in range(2)]
        y_all_b = [Buf(), Buf()]
        for q in range(2):
            _gather(C, y_own[q], y_own_b[q], y_all[q], y_all_b[q])
        if final:
            dst, dst_b = [(outT, 0, 8)], []
        else:
            hx_own = [nc.dram_tensor("hx_own%d_%d" % (L, q), [256, 2048], F32, kind="Internal").ap() for q in range(4)]
            hx_own_b = [Buf() for _ in range(4)]
            dst = [(hx_own[q].rearrange("(c p) t -> p c t", p=128), 2 * q, 2 * q + 2) for q in range(4)]
            dst_b = hx_own_b
        chan_body(C, pre, "ffn" if even else "moe", final, hprev, hprev_b, None,
                  [y_all[q].rearrange("(c p) t -> p c t", p=128) for q in range(2)], rsel, y_all_b, dst, dst_b)
        stage += 1
        if stage >= upto and not final:
            dbg = C.dout("dbg_h", [1024, 2048])
            for q in range(4):
                C.dma(dbg[q * 256:(q + 1) * 256], hx_own[q], hx_own_b, [])
            break
        if not final:
            hx_all = [nc.dram_tensor("hx_all%d_%d" % (L, q), [512, 2048], F32, kind="Internal").ap() for q in range(4)]
            hx_all_b = [Buf() for _ in range(4)]
            for q in range(4):
                _gather(C, hx_own[q], hx_own_b[q], hx_all[q], hx_all_b[q])
            hprev, hprev_b = dst, dst_b
    return C.close()


def fused_inputs(inp, c, upto=8):
    b, r = c // 2, c % 2
    xT = np.ascontiguousarray(inp["x"][b].T)
    m = {"x_full": xT, "x_own": np.ascontiguousarray(xT[:, r * 2048:(r + 1) * 2048])}
    rs = np.zeros((128, 2), np.float32); rs[:, r] = 1.0
    m["rsel"] = rs
    stage = 0
    for L in range(4):
        pre = "L%d_" % L
        j = L // 2
        if L % 2 == 0:
            m.update(mix_even_inputs(inp, j, L, b, r, None, pre))
        else:
            m.update(mix_odd_inputs(inp, j, L, b, r, None, pre))
        stage += 1
        if stage >= upto:
            break
        m.update(chan_inputs(inp, L, None, None, L == 3, pre, perm=True))
        stage += 1
        if stage >= upto:
            break
    m.update(const_inputs())
    return m


def kernel(**inp):
    inp = {k: np.asarray(v) for k, v in inp.items()}
    nc = build_fused()
    maps = [fused_inputs(inp, c) for c in range(8)]
    res = run_bass_kernel_spmd(nc, maps, core_ids=list(range(8)))
    out = np.empty((4, 4096, 1024), np.float32)
    for c in range(8):
        b, r = c // 2, c % 2
        out[b, r * 2048:(r + 1) * 2048, :] = res.results[c]["outT"].T
    return out
```

```python
import numpy as np
import concourse.bass as bass
import concourse.mybir as mybir
from concourse.bass_utils import run_bass_kernel_spmd
from contextlib import ExitStack

F32 = mybir.dt.float32
BF16 = mybir.dt.bfloat16
AF = mybir.ActivationFunctionType
ALU = mybir.AluOpType
AX = mybir.AxisListType

ENGS = ("pe", "act", "dve", "pool", "sp")
EIDX = {e: i for i, e in enumerate(ENGS)}


class Buf:
    __slots__ = ("w", "r")

    def __init__(self):
        self.w = None
        self.r = []


class Op:
    __slots__ = ("eng", "fn", "is_dma", "deps", "k", "inc", "dsem", "dval", "clock", "waited", "cinc")

    def __init__(self, eng, fn, is_dma):
        self.eng = eng
        self.fn = fn
        self.is_dma = is_dma
        self.deps = []
        self.k = 0
        self.inc = 0
        self.dsem = None
        self.dval = 0
        self.clock = None
        self.waited = False
        self.cinc = 16


class Sched:
    def __init__(self, nc, stack, n_dma_sems=40):
        self.nc = nc
        self.eng = {"pe": nc.tensor, "act": nc.scalar, "dve": nc.vector,
                    "pool": nc.gpsimd, "sp": nc.sync}
        self.ops = []
        self.sems = {e: stack.enter_context(nc.semaphore("s_" + e)) for e in ENGS}
        self.dsems = [stack.enter_context(nc.semaphore("d%d" % i)) for i in range(n_dma_sems)]
        self.cnt = {e: 0 for e in ENGS}
        self.last_clock = {e: [0] * len(ENGS) for e in ENGS}
        self.dma_known = {e: set() for e in ENGS}
        self.inc_count = {e: 0 for e in ENGS}
        nd = n_dma_sems
        self.dcount = [0] * nd
        self.dprev = [None] * nd
        self.di = 0
        self.total = 0

    def op(self, eng, fn, reads=(), writes=(), dma=False):
        o = Op(eng, fn, dma)
        deps = set()
        for b in reads:
            if b.w is not None:
                deps.add(b.w)
        for b in writes:
            if b.w is not None:
                deps.add(b.w)
            for r in b.r:
                deps.add(r)
        o.deps = list(deps)
        for b in reads:
            b.r.append(o)
        for b in writes:
            b.w = o
            b.r = []
        self.ops.append(o)
        return o

    def dma(self, fn, reads=(), writes=(), eng="sp"):
        return self.op(eng, fn, reads, writes, dma=True)

    def emit(self, barrier=True):
        ops = self.ops
        self.ops = []
        self.total += len(ops)
        nE = len(ENGS)
        for o in ops:
            self.cnt[o.eng] += 1
            o.k = self.cnt[o.eng]
        needed = []
        for o in ops:
            clk = list(self.last_clock[o.eng])
            known = self.dma_known[o.eng]
            waits = []
            for d in sorted(o.deps, key=lambda d: -d.k):
                if d.is_dma:
                    if d.k == 0 or d in known:
                        continue
                    known.add(d)
                    waits.append(d)
                    d.waited = True
                    dc = d.clock
                    for i in range(nE):
                        if dc[i] > clk[i]:
                            clk[i] = dc[i]
                else:
                    j = EIDX[d.eng]
                    if d.eng == o.eng and o.eng == "pe":
                        continue
                    if clk[j] >= d.k:
                        continue
                    waits.append(d)
                    d.waited = True
                    dc = d.clock
                    for i in range(nE):
                        if dc[i] > clk[i]:
                            clk[i] = dc[i]
                    if clk[j] < d.k:
                        clk[j] = d.k
            self.last_clock[o.eng] = clk
            oc = list(clk)
            if not o.is_dma:
                oc[EIDX[o.eng]] = o.k
            o.clock = oc
            needed.append(waits)
        last_of = {}
        if barrier:
            for o in ops:
                if not o.is_dma:
                    last_of[o.eng] = o
            for o in last_of.values():
                o.waited = True
        nd = len(self.dsems)
        for o in ops:
            if o.is_dma:
                s = self.di % nd
                self.di += 1
                o.dsem = s
                self.dcount[s] += o.cinc
                o.dval = self.dcount[s]
            elif o.waited:
                self.inc_count[o.eng] += 1
                o.inc = self.inc_count[o.eng]
        for o, waits in zip(ops, needed):
            e = self.eng[o.eng]
            if o.is_dma and self.dprev[o.dsem] is not None:
                p = self.dprev[o.dsem]
                e.wait_ge(self.dsems[p.dsem], p.dval)
            for d in waits:
                if d.is_dma:
                    e.wait_ge(self.dsems[d.dsem], d.dval)
                else:
                    e.wait_ge(self.sems[d.eng], d.inc)
            ins = o.fn()
            if o.is_dma:
                ins.then_inc(self.dsems[o.dsem], o.cinc)
                self.dprev[o.dsem] = o
            elif o.waited:
                ins.then_inc(self.sems[o.eng], 1)
        if barrier:
            for en in ENGS:
                e = self.eng[en]
                for e2 in ENGS:
                    if self.inc_count[e2] > 0 and e2 != "sp":
                        e.wait_ge(self.sems[e2], self.inc_count[e2])
                for p in self.dprev:
                    if p is not None:
                        e.wait_ge(self.dsems[p.dsem], p.dval)
                self.last_clock[en] = [self.cnt[x] for x in ENGS]
            for o in ops:
                o.k = 0


class Ctx:
    def __init__(self):
        self.nc = bass.Bass("TRN2", target_bir_lowering=False)
        self.st = ExitStack()
        self.S = Sched(self.nc, self.st)
        self.n = 0
        self.E = self.S.eng

    def din(self, name, shape, dt=F32):
        return self.nc.dram_tensor(name, list(shape), dt, kind="ExternalInput").ap()

    def dout(self, name, shape, dt=F32):
        return self.nc.dram_tensor(name, list(shape), dt, kind="ExternalOutput").ap()

    def sb(self, shape, dt, st=None):
        self.n += 1
        return (st or self.st).enter_context(self.nc.sbuf_tensor("t%d" % self.n, list(shape), dt))

    def ps(self, shape=(128, 512), dt=F32, st=None):
        self.n += 1
        return (st or self.st).enter_context(self.nc.psum_tensor("p%d" % self.n, list(shape), dt))

    def op(self, eng, method, reads, writes, *a, **kw):
        e = self.E[eng]
        return self.S.op(eng, lambda: getattr(e, method)(*a, **kw), reads, writes)

    def dma(self, out, in_, reads, writes, eng="sp"):
        e = self.E[eng]
        return self.S.dma(lambda: e.dma_start(out=out, in_=in_), reads, writes, eng=eng)

    def close(self):
        self.S.emit(barrier=True)
        self.st.close()
        return self.nc


class Rot:
    def __init__(self, items):
        self.items = items
        self.i = 0

    def next(self):
        it = self.items[self.i % len(self.items)]
        self.i += 1
        return it


S_LEN = 4096
NB = S_LEN // 512
EPS = 1e-6


def load_consts(C, names=("ident", "e127", "tri", "bdtri", "sel")):
    out = {}
    if not hasattr(C, "cdram"):
        C.cdram = {}
    for nm in names:
        shape = [8, 1024] if nm == "sel" else [128, 128]
        if nm not in C.cdram:
            C.cdram[nm] = C.din("c_" + nm, shape)
        d = C.cdram[nm]
        t = C.sb(shape, F32)
        b = Buf()
        C.dma(t[:], d, [], [b])
        out[nm] = (t, b)
    return out


def make_misc(C):
    ones_bf = C.sb([128, 128], BF16); b1 = Buf()
    C.op("pool", "memset", [], [b1], ones_bf[:], 1.0)
    cols = C.sb([128, 4], F32); b2 = Buf()
    C.op("pool", "memset", [], [b2], cols[:, 0:1], EPS)
    C.op("pool", "memset", [], [b2], cols[:, 1:2], 1.0)
    C.op("pool", "memset", [], [b2], cols[:, 2:3], 0.0)
    ones_f = C.sb([128, 512], F32); b3 = Buf()
    C.op("pool", "memset", [], [b3], ones_f[:], 1.0)
    return dict(ones_bf=(ones_bf, b1), cols=(cols, b2), ones_f=(ones_f, b3))


def load_w_bf16(C, dst, dst_buf, src_view, ncols, stage, stage_buf, eng_cycle=("dve", "pool"), step=512):
    i = 0
    for c0 in range(0, ncols, step):
        n = min(step, ncols - c0)
        C.dma(stage[:, :, 0:n], src_view[:, :, c0:c0 + n], [], [stage_buf])
        eng = eng_cycle[i % len(eng_cycle)]
        i += 1
        C.op(eng, "tensor_copy", [stage_buf], [dst_buf], out=dst[:, :, c0:c0 + n], in_=stage[:, :, 0:n])


def norm_block(C, M, hb, hb_buf, g, g_buf, hn, hn_buf, sq, sq_buf, rstd, rstd_buf, pst, ps_buf, n=512, nch=8,
               hn32=None, hn32_buf=None, inv_d=1.0 / 1024):
    ones_bf, ob = M["ones_bf"]
    cols, cb = M["cols"]
    C.op("act", "activation", [hb_buf], [sq_buf], out=sq[:, 0:nch, 0:n], in_=hb[:, 0:nch, 0:n], func=AF.Square)
    for c in range(nch):
        C.op("pe", "matmul", [ob, sq_buf], [ps_buf], pst[:, 0:n], lhsT=ones_bf[:], rhs=sq[:, c, 0:n],
             start=(c == 0), stop=(c == nch - 1))
    C.op("act", "activation", [ps_buf, cb], [rstd_buf], out=rstd[:, 0:n], in_=pst[:, 0:n], func=AF.Ln,
         bias=cols[:, 0:1], scale=inv_d)
    C.op("act", "activation", [rstd_buf], [rstd_buf], out=rstd[:, 0:n], in_=rstd[:, 0:n], func=AF.Exp, scale=-0.5)
    for c in range(nch):
        eng = "dve"
        C.op(eng, "scalar_tensor_tensor", [hb_buf, g_buf, rstd_buf], [hn_buf], out=hn[:, c, 0:n], in0=hb[:, c, 0:n],
             scalar=g[:, c:c + 1], in1=rstd[:, 0:n], op0=ALU.mult, op1=ALU.mult)
        if hn32 is not None:
            eng2 = "dve"
            C.op(eng2, "scalar_tensor_tensor", [hb_buf, g_buf, rstd_buf], [hn32_buf], out=hn32[:, c, 0:n],
                 in0=hb[:, c, 0:n], scalar=g[:, c:c + 1], in1=rstd[:, 0:n], op0=ALU.mult, op1=ALU.mult)


def build_mix_even(phases=(1, 2, 3)):
    C = Ctx()
    hT = C.din("hT", [1024, S_LEN]).rearrange("(c p) t -> p c t", p=128)
    yT = C.dout("yT", [512, S_LEN], BF16)
    mix_even_body(C, "", lambda blk: [(hT[:, :, blk * 512:(blk + 1) * 512], 0, 8)], lambda r0, r1: yT[r0:r1], [], [], phases)
    return C.close()


def mix_even_body(C, pre, hblk, yrow, h_rd, y_wr, phases=(1, 2, 3)):
    old_st = C.st
    C.st = ExitStack()
    nc = C.nc
    gmix = C.din(pre + "gmix", [128, 8])
    w = C.din(pre + "w", [1024, 1284]).rearrange("(c p) n -> p c n", p=128)
    convw = C.din(pre + "convw", [128, 2, 4])
    vecs = C.din(pre + "vecs", [128, 2, 4])
    gaw = C.din(pre + "gaw", [4, 64, 64])
    gxw = C.din(pre + "gxw", [4, 64, 64])
    fb = C.din(pre + "fb", [4, 1])

    K = load_consts(C, ("ident", "e127", "tri"))
    M = make_misc(C)
    cols, cb = M["cols"]

    Wb = C.sb([128, 8, 1284], BF16); Wb_b = Buf()
    g_t = C.sb([128, 8], F32); g_b = Buf()
    C.dma(g_t[:], gmix, [], [g_b])
    cw_t = C.sb([128, 2, 4], F32); cw_b = Buf()
    C.dma(cw_t[:], convw, [], [cw_b])
    vc_t = C.sb([128, 2, 4], F32); vc_b = Buf()
    C.dma(vc_t[:], vecs, [], [vc_b])
    fb_t = C.sb([4, 1], F32); fb_b = Buf()
    C.dma(fb_t[:], fb, [], [fb_b])

    qT = C.sb([128, 2, S_LEN], BF16); q_b = [[Buf() for _ in range(NB)] for _ in range(2)]
    kT = C.sb([128, 2, S_LEN], BF16); k_b = [[Buf() for _ in range(NB)] for _ in range(2)]
    vx = C.sb([128, 32, 4, 65], BF16); v_b = [Buf() for _ in range(32)]
    fT = C.sb([4, S_LEN], F32); f_b = Buf()

    pxy = ExitStack()
    xrT = C.sb([128, 2, S_LEN], F32, pxy); xr_b = [[Buf() for _ in range(NB)] for _ in range(2)]
    yrT = C.sb([128, 2, S_LEN], F32, pxy); yr_b = [[Buf() for _ in range(NB)] for _ in range(2)]
    C.op("pool", "memset", [], v_b, vx[:, :, :, 64:65], 1.0)

    with ExitStack() as p1:
        hb = [C.sb([128, 8, 512], F32, p1) for _ in range(2)]; hb_b = [Buf(), Buf()]
        load_w_bf16(C, Wb, Wb_b, w, 1284, hb[1], hb_b[1])
        sq = C.sb([128, 8, 512], BF16, p1); sq_b = Buf()
        rstd = C.sb([128, 512], F32, p1); rstd_b = Buf()
        hn = C.sb([128, 8, 512], BF16, p1); hn_b = Buf()
        pss = Rot([(C.ps(st=p1), Buf()) for _ in range(8)])
        for blk in range(NB):
            t0 = blk * 512
            h_t, h_b = hb[blk % 2], hb_b[blk % 2]
            for (src_, lo_, hi_) in hblk(blk):
                C.dma(h_t[:, lo_:hi_, :], src_, h_rd, [h_b])
            pst, psb = pss.next()
            norm_block(C, M, h_t, h_b, g_t, g_b, hn, hn_b, sq, sq_b, rstd, rstd_b, pst, psb)
            for oc in range(8):
                pst, psb = pss.next()
                for kc in range(8):
                    C.op("pe", "matmul", [Wb_b, hn_b], [psb], pst[:], lhsT=Wb[:, kc, oc * 128:(oc + 1) * 128],
                         rhs=hn[:, kc, :], start=(kc == 0), stop=(kc == 7))
                if oc < 2:
                    C.op("act", "copy", [psb], [xr_b[oc][blk]], out=xrT[:, oc, t0:t0 + 512], in_=pst[:])
                elif oc < 4:
                    C.op("dve", "tensor_copy", [psb], [yr_b[oc - 2][blk]], out=yrT[:, oc - 2, t0:t0 + 512], in_=pst[:])
                elif oc < 6:
                    C.op("act", "copy", [psb], [q_b[oc - 4][blk]], out=qT[:, oc - 4, t0:t0 + 512], in_=pst[:])
                else:
                    C.op("dve", "tensor_copy", [psb], [k_b[oc - 6][blk]], out=kT[:, oc - 6, t0:t0 + 512], in_=pst[:])
            pst, psb = pss.next()
            for kc in range(8):
                C.op("pe", "matmul", [Wb_b, hn_b], [psb], pst[0:4, :], lhsT=Wb[:, kc, 1280:1284],
                     rhs=hn[:, kc, :], start=(kc == 0), stop=(kc == 7))
            C.op("act", "copy", [psb], [f_b], out=fT[:, t0:t0 + 512], in_=pst[0:4, :])
            for sub in range(4):
                pst, psb = pss.next()
                for kc in range(8):
                    C.op("pe", "matmul", [Wb_b, hn_b], [psb], pst[:, 0:256], lhsT=hn[:, kc, sub * 128:(sub + 1) * 128],
                         rhs=Wb[:, kc, 1024:1280], start=(kc == 0), stop=(kc == 7))
                tb = blk * 4 + sub
                C.op("dve" if sub % 2 else "act", "tensor_copy" if sub % 2 else "copy", [psb], [v_b[tb]],
                     out=vx[:, tb, :, 0:64], in_=pst[:, 0:256].rearrange("p (h d) -> p h d", h=4))
        C.S.emit(barrier=True)

    with ExitStack() as p2:
      if 2 in phases:
          TB = 1024
          wst = C.sb([128, 2, 2, 128], F32, p2); wst_b = Buf()
          C.op("pool", "memset", [], [wst_b], wst[:], 0.0)
          for c in range(2):
              for n in range(2):
                  blkid = c * 2 + n
                  C.dma(wst[n * 64:(n + 1) * 64, c, 0, n * 64:(n + 1) * 64], gaw[blkid], [], [wst_b])
                  C.dma(wst[n * 64:(n + 1) * 64, c, 1, n * 64:(n + 1) * 64], gxw[blkid], [], [wst_b])
          wbd = C.sb([128, 2, 2, 128], BF16, p2); wbd_b = Buf()
          C.op("dve", "tensor_copy", [wst_b], [wbd_b], out=wbd[:], in_=wst[:])
          sc = C.sb([128, 2, 4], F32, p2); sc_b = Buf()
          C.op("act", "activation", [vc_b], [sc_b], out=sc[:, :, 0], in_=vc_t[:, :, 3], func=AF.Exp, scale=-1.0)
          C.op("act", "activation", [sc_b, cb], [sc_b], out=sc[:, :, 1], in_=sc[:, :, 0], func=AF.Ln, bias=cols[:, 1:2])
          C.op("dve", "tensor_scalar", [sc_b], [sc_b], out=sc[:, :, 2], in0=sc[:, :, 1], scalar1=-8.0, scalar2=None, op0=ALU.mult)
          C.op("dve", "tensor_scalar", [sc_b], [sc_b], out=sc[:, :, 3], in0=sc[:, :, 1], scalar1=-16.0, scalar2=None, op0=ALU.mult)

          def T(dt=F32):
              return C.sb([128, TB], dt, p2), Buf()
          xc, xc_b = T(); xcb, xcb_b = T(BF16); r_t, r_b = T(); i_t, i_b = T(); a_t, a_b = T(); s_t, s_b = T()
          u_t, u_b = T(); g1, g1_b = T(); g2, g2_b = T(); yo, yo_b = T(BF16)
          hsc = [C.sb([128, TB], F32, p2) for _ in range(2)]; hsc_b = [Buf(), Buf()]
          pss = Rot([(C.ps(st=p2), Buf()) for _ in range(4)])
          for c in range(2):
              allx = [xr_b[c][b] for b in range(NB)]
              ally = [yr_b[c][b] for b in range(NB)]
              for tb in range(S_LEN // TB):
                  t0 = tb * TB
                  C.op("dve", "tensor_scalar", allx + [cw_b, vc_b], [xc_b], out=xc[:], in0=xrT[:, c, t0:t0 + TB],
                       scalar1=cw_t[:, c, 3:4], scalar2=vc_t[:, c, 0:1], op0=ALU.mult, op1=ALU.add)
                  for d in (1, 2, 3):
                      o0 = d if t0 == 0 else 0
                      eng = "dve"
                      C.op(eng, "scalar_tensor_tensor", allx + [cw_b, xc_b], [xc_b], out=xc[:, o0:TB],
                           in0=xrT[:, c, t0 + o0 - d:t0 + TB - d], scalar=cw_t[:, c, 3 - d:4 - d], in1=xc[:, o0:TB],
                           op0=ALU.mult, op1=ALU.add)
                  C.op("act", "copy", [xc_b], [xcb_b], out=xcb[:], in_=xc[:])
                  for half in range(TB // 512):
                      sl = slice(half * 512, (half + 1) * 512)
                      pa, pab = pss.next()
                      C.op("pe", "matmul", [wbd_b, xcb_b], [pab], pa[:], lhsT=wbd[:, c, 0, :], rhs=xcb[:, sl], start=True, stop=True)
                      C.op("act", "activation", [pab, vc_b], [r_b], out=r_t[:, sl], in_=pa[:], func=AF.Sigmoid, bias=vc_t[:, c, 1:2])
                      px, pxb = pss.next()
                      C.op("pe", "matmul", [wbd_b, xcb_b], [pxb], px[:], lhsT=wbd[:, c, 1, :], rhs=xcb[:, sl], start=True, stop=True)
                      C.op("act", "activation", [pxb, vc_b], [i_b], out=i_t[:, sl], in_=px[:], func=AF.Sigmoid, bias=vc_t[:, c, 2:3])
                  C.op("act", "activation", [r_b, sc_b], [a_b], out=a_t[:], in_=r_t[:], func=AF.Exp, scale=sc[:, c, 2:3])
                  C.op("act", "activation", [r_b, sc_b], [s_b], out=s_t[:], in_=r_t[:], func=AF.Exp, scale=sc[:, c, 3:4])
                  C.op("act", "activation", [s_b, cb], [s_b], out=s_t[:], in_=s_t[:], func=AF.Sqrt, scale=-1.0, bias=cols[:, 1:2])
                  C.op("pool", "tensor_tensor", [i_b, xc_b], [u_b], out=u_t[:], in0=i_t[:], in1=xc[:], op=ALU.mult)
                  C.op("dve", "tensor_tensor", [u_b, s_b], [u_b], out=u_t[:], in0=u_t[:], in1=s_t[:], op=ALU.mult)
                  hcur, hcur_b = hsc[tb % 2], hsc_b[tb % 2]
                  hprev, hprev_b = hsc[(tb + 1) % 2], hsc_b[(tb + 1) % 2]
                  if tb == 0:
                      C.op("dve", "tensor_tensor_scan", [a_b, u_b], [hcur_b], out=hcur[:], data0=a_t[:], data1=u_t[:],
                           initial=0.0, op0=ALU.mult, op1=ALU.add)
                  else:
                      C.op("dve", "tensor_tensor_scan", [a_b, u_b, hprev_b], [hcur_b], out=hcur[:], data0=a_t[:], data1=u_t[:],
                           initial=hprev[:, TB - 1:TB], op0=ALU.mult, op1=ALU.add)
                  yv = yrT[:, c, t0:t0 + TB]
                  C.op("act", "activation", ally, [g1_b], out=g1[:], in_=yv, func=AF.Square)
                  C.op("pool", "tensor_scalar", [g1_b], [g1_b], out=g1[:], in0=g1[:], scalar1=0.044715, scalar2=1.0,
                       op0=ALU.mult, op1=ALU.add)
                  C.op("pool", "tensor_tensor", ally + [g1_b], [g1_b], out=g1[:], in0=g1[:], in1=yv, op=ALU.mult)
                  C.op("act", "activation", [g1_b], [g2_b], out=g2[:], in_=g1[:], func=AF.Sigmoid, scale=1.5957691216057308)
                  C.op("pool", "tensor_tensor", ally + [g2_b], [g2_b], out=g2[:], in0=g2[:], in1=yv, op=ALU.mult)
                  C.op("dve", "tensor_tensor", [g2_b, hcur_b], [yo_b], out=yo[:], in0=g2[:], in1=hcur[:], op=ALU.mult)
                  C.dma(yrow(c * 128, (c + 1) * 128)[:, t0:t0 + TB], yo[:], [yo_b], y_wr)
          C.S.emit(barrier=True)

    pxy.close()
    with ExitStack() as p3:
      if 3 in phases:
          ident, id_b = K["ident"]; e127, e_b = K["e127"]; tri, tri_b = K["tri"]
          ones_f, of_b = M["ones_f"]
          tri_bf = C.sb([128, 128], BF16, p3); trb_b = Buf()
          C.op("dve", "tensor_copy", [tri_b], [trb_b], out=tri_bf[:], in_=tri[:])
          nfb = C.sb([4, 1], F32, p3); nfb_b = Buf()
          C.op("dve", "tensor_scalar", [fb_b], [nfb_b], out=nfb[:], in0=fb_t[:], scalar1=-1.0, scalar2=None, op0=ALU.mult)
          sp = C.sb([4, S_LEN], F32, p3); sp_b = Buf()
          C.op("act", "activation", [f_b, nfb_b], [sp_b], out=sp[:], in_=fT[:], func=AF.Exp, scale=-1.0, bias=nfb[:, 0:1])
          C.op("act", "activation", [sp_b, cb], [sp_b], out=sp[:], in_=sp[:], func=AF.Ln, bias=cols[0:4, 1:2])
          cpos = C.sb([4, S_LEN], F32, p3); cpos_b = Buf()
          for j in range(8):
              sl = slice(j * 512, (j + 1) * 512)
              init = 0.0 if j == 0 else cpos[:, j * 512 - 1:j * 512]
              C.op("dve", "tensor_tensor_scan", [sp_b, of_b, cpos_b], [cpos_b], out=cpos[:, sl], data0=ones_f[0:4, :],
                   data1=sp[:, sl], initial=init, op0=ALU.mult, op1=ALU.add)
          pcc = C.ps(st=p3); pcc_b = Buf()
          for j in range(32):
              C.op("pe", "matmul", [cpos_b, id_b], [pcc_b], pcc[:, j * 4:(j + 1) * 4], lhsT=cpos[:, j * 128:(j + 1) * 128],
                   rhs=ident[0:4, 0:4], start=True, stop=True)
          ccol = C.sb([128, 32, 4], F32, p3); ccol_b = Buf()
          C.op("dve", "tensor_copy", [pcc_b], [ccol_b], out=ccol[:], in_=pcc[:, 0:128].rearrange("p (j h) -> p j h", h=4))
          pbc = C.ps(st=p3); pbc_b = Buf()
          C.op("pe", "matmul", [ccol_b, e_b], [pbc_b], pbc[:, 0:128], lhsT=e127[:], rhs=ccol[:].rearrange("p j h -> p (j h)"),
               start=True, stop=True)
          bcv = C.sb([128, 32, 4], F32, p3); bcv_b = Buf()
          C.op("dve", "tensor_copy", [pbc_b], [bcv_b], out=bcv[:], in_=pbc[:, 0:128].rearrange("p (j h) -> p j h", h=4))
          tab = C.sb([128, 4, 32, 32], F32, p3); tab_b = Buf()
          for h in range(4):
              for j in range(32):
                  eng = "dve" if j % 2 == 0 else "pool"
                  C.op(eng, "tensor_scalar", [bcv_b, ccol_b], [tab_b], out=tab[:, h, j, :], in0=bcv[:, :, h],
                       scalar1=ccol[:, j, h:h + 1], scalar2=-1.0, op0=ALU.subtract, op1=ALU.mult)
          pS = Rot([(C.ps(st=p3), Buf()) for _ in range(3)])
          pO = Rot([(C.ps(st=p3), Buf()) for _ in range(2)])
          pB = C.ps(st=p3); pB_b = Buf()
          PT = Rot([(C.sb([128, 512], BF16, p3), Buf()) for _ in range(3)])
          osb = Rot([(C.sb([128, 512], F32, p3), Buf()) for _ in range(2)])
          rden = C.sb([128, 512], F32, p3); rden_b = Buf()
          yb = Rot([(C.sb([128, 512], BF16, p3), Buf()) for _ in range(2)])
          allq = lambda p_: [q_b[p_][b] for b in range(NB)]
          for h in range(4):
              p_ = h // 2
              pb = (h % 2) * 64
              for qb in range(8):
                  O, O_b = pO.next()
                  nk = 4 * qb + 4
                  for kb in range(nk):
                      i0 = max(kb - 4 * qb, 0)
                      qs = 512 * qb + 128 * i0
                      n = 512 - 128 * i0
                      ST, ST_b = pS.next()
                      C.op("pe", "matmul", [k_b[p_][kb // 4], q_b[p_][qb]], [ST_b], ST[:, 0:n],
                           lhsT=kT[pb:pb + 64, p_, kb * 128:(kb + 1) * 128], rhs=qT[pb:pb + 64, p_, qs:qs + n],
                           start=True, stop=True)
                      P, P_b = PT.next()
                      for i in range(i0, 4):
                          C.op("act", "activation", [ST_b, tab_b], [P_b], out=P[:, i * 128:(i + 1) * 128],
                               in_=ST[:, (i - i0) * 128:(i - i0 + 1) * 128], func=AF.Exp, scale=0.125,
                               bias=tab[:, h, kb, 4 * qb + i:4 * qb + i + 1])
                      if kb >= 4 * qb:
                          C.op("pool", "tensor_tensor", [P_b, trb_b], [P_b], out=P[:, i0 * 128:(i0 + 1) * 128],
                               in0=P[:, i0 * 128:(i0 + 1) * 128], in1=tri_bf[:], op=ALU.mult)
                      C.op("pe", "matmul", [v_b[kb], P_b], [O_b], O[0:65, 128 * i0:512], lhsT=vx[:, kb, h, :],
                           rhs=P[:, 128 * i0:512], start=(kb == 0), stop=(kb == nk - 1))
                  C.op("dve", "reciprocal", [O_b], [rden_b], out=rden[64:65, :], in_=O[64:65, :])
                  C.op("pe", "matmul", [rden_b, of_b], [pB_b], pB[0:64, :], lhsT=ones_f[64:65, 0:64], rhs=rden[64:65, :],
                       start=True, stop=True)
                  o_s, o_sb = osb.next()
                  C.op("act", "copy", [O_b], [o_sb], out=o_s[0:64, :], in_=O[0:64, :])
                  y_t, y_tb = yb.next()
                  C.op("dve", "tensor_tensor", [o_sb, pB_b], [y_tb], out=y_t[0:64, :], in0=o_s[0:64, :], in1=pB[0:64, :], op=ALU.mult)
                  C.dma(yrow(256 + h * 64, 256 + (h + 1) * 64)[:, qb * 512:(qb + 1) * 512], y_t[0:64, :], [y_tb], y_wr)
          C.S.emit(barrier=True)
    C.S.emit(barrier=True)
    C.st.close()
    C.st = old_st


def mix_even_inputs(inp, j, layer, b, g, hT_full, pre=""):
    W = inp["ev_w_in"][j]
    cs = lambda base, n: W[:, base + g * n: base + (g + 1) * n]
    w = np.concatenate([cs(0, 256), cs(512, 256), cs(1024, 256), cs(1536, 256), cs(2048, 256),
                        W[:, 2560 + g * 4:2560 + (g + 1) * 4]], axis=1)
    ch = slice(g * 256, (g + 1) * 256)
    pc = lambda v: np.ascontiguousarray(v.reshape(2, 128).T)
    convw = np.ascontiguousarray(inp["ev_conv_w"][j][:, ch].reshape(4, 2, 128).transpose(2, 1, 0))
    vecs = np.stack([pc(inp["ev_conv_b"][j][ch]), pc(inp["ev_ga_b"][j][ch]), pc(inp["ev_gx_b"][j][ch]),
                     pc(inp["ev_lambda"][j][ch])], axis=2)
    d = {
        "gmix": np.ascontiguousarray(inp["norm_mix"][layer].reshape(8, 128).T),
        "w": np.ascontiguousarray(w),
        "convw": convw, "vecs": np.ascontiguousarray(vecs),
        "gaw": np.ascontiguousarray(inp["ev_ga_w"][j][g * 4:(g + 1) * 4]),
        "gxw": np.ascontiguousarray(inp["ev_gx_w"][j][g * 4:(g + 1) * 4]),
        "fb": np.ascontiguousarray(inp["ev_f_b"][j][g * 4:(g + 1) * 4].reshape(4, 1)),
    }
    d = {pre + k: v for k, v in d.items()}
    if hT_full is not None:
        d["hT"] = np.ascontiguousarray(hT_full)
    return d


def const_inputs():
    ident = np.eye(128, dtype=np.float32)
    e127 = np.zeros((128, 128), np.float32); e127[127, :] = 1.0
    k = np.arange(128)
    tri = (k[:, None] <= k[None, :]).astype(np.float32)
    bdtri = tri * ((k[:, None] // 64) == (k[None, :] // 64))
    sel = np.zeros((8, 8, 128), np.float32)
    for e in range(8):
        sel[e, e, :] = 1.0
    return {"c_ident": ident, "c_e127": e127, "c_tri": tri, "c_bdtri": bdtri.astype(np.float32),
            "c_sel": sel.reshape(8, 1024)}


NT = 2048
NBC = NT // 512


def build_chan(kind, final):
    C = Ctx()
    hT = C.din("hT", [1024, NT]).rearrange("(c p) t -> p c t", p=128)
    yT = C.din("yT", [1024, NT], BF16).rearrange("(c p) t -> p c t", p=128)
    outT = C.dout("outT", [1024, NT]).rearrange("(c p) t -> p c t", p=128)
    chan_body(C, "", kind, final, [(hT, 0, 8)], [], yT, None, None, [], [(outT, 0, 8)], [])
    return C.close()


def chan_body(C, pre, kind, final, hT, h_rd, yT, y_all, rsel_d, y_rd, outT, out_wr):
    old_st = C.st
    C.st = ExitStack()
    wout = C.din(pre + "wout", [1024, 1024]).rearrange("(c p) n -> p c n", p=128)
    gffn = C.din(pre + "gffn", [128, 8])
    if kind == "ffn":
        DFF, NE = 2816, 1
        w1 = [C.din(pre + "w1", [1024, DFF]).rearrange("(c p) n -> p c n", p=128)]
        w3 = [C.din(pre + "w3", [1024, DFF]).rearrange("(c p) n -> p c n", p=128)]
        w2 = [C.din(pre + "w2", [DFF, 1024]).rearrange("(c p) n -> p c n", p=128)]
    else:
        DFF, NE = 3584, 8
        router = C.din(pre + "router", [1024, 8]).rearrange("(c p) n -> p c n", p=128)
        ew1 = C.din(pre + "ew1", [8, 1024, DFF]); ew3 = C.din(pre + "ew3", [8, 1024, DFF]); ew2 = C.din(pre + "ew2", [8, DFF, 1024])
        w1 = [ew1[e].rearrange("(c p) n -> p c n", p=128) for e in range(8)]
        w3 = [ew3[e].rearrange("(c p) n -> p c n", p=128) for e in range(8)]
        w2 = [ew2[e].rearrange("(c p) n -> p c n", p=128) for e in range(8)]
    if final:
        gfin = C.din(pre + "gfin", [128, 8])
    NFG = DFF // 256

    K = load_consts(C, ("ident", "sel") if kind == "moe" else ())
    M = make_misc(C)
    cols, cb = M["cols"]

    h = C.sb([128, 8, NT], F32); h_b = [[Buf() for _ in range(NBC)] for _ in range(8)]
    g_t = C.sb([128, 8], F32); g_b = Buf()
    C.dma(g_t[:], gffn, [], [g_b])
    if final:
        gf_t = C.sb([128, 8], F32); gf_b = Buf()
        C.dma(gf_t[:], gfin, [], [gf_b])
    for blk in range(NBC):
        for (src_, lo_, hi_) in hT:
            C.dma(h[:, lo_:hi_, blk * 512:(blk + 1) * 512], src_[:, :, blk * 512:(blk + 1) * 512], h_rd, [h_b[c][blk] for c in range(lo_, hi_)])

    with ExitStack() as p1:
        y = C.sb([128, 8, NT], BF16, p1); y_b = [Buf() for _ in range(NBC)]
        if y_all is None:
            for blk in range(NBC):
                C.dma(y[:, :, blk * 512:(blk + 1) * 512], yT[:, :, blk * 512:(blk + 1) * 512], y_rd, [y_b[blk]])
        else:
            rs_t = C.sb([128, 2], F32, p1); rs_b = Buf()
            C.dma(rs_t[:], rsel_d, [], [rs_b])
            yA = Rot([(C.sb([128, 8, 512], BF16, p1), Buf()) for _ in range(2)])
            yB = Rot([(C.sb([128, 8, 512], BF16, p1), Buf()) for _ in range(2)])
            for blk in range(NBC):
                sl = slice(blk * 512, (blk + 1) * 512)
                a_t, a_b = yA.next()
                b_t, b_b = yB.next()
                for qi, yv in enumerate(y_all):
                    C.dma(a_t[:, 4 * qi:4 * qi + 4, :], yv[:, :, blk * 512:(blk + 1) * 512], y_rd, [a_b])
                    C.dma(b_t[:, 4 * qi:4 * qi + 4, :], yv[:, :, NT + blk * 512:NT + (blk + 1) * 512], y_rd, [b_b])
                C.op("dve", "tensor_scalar", [a_b, rs_b], [a_b], out=a_t[:], in0=a_t[:], scalar1=rs_t[:, 0:1], scalar2=None, op0=ALU.mult)
                C.op("dve", "scalar_tensor_tensor", [a_b, b_b, rs_b], [y_b[blk]], out=y[:, :, sl], in0=b_t[:], scalar=rs_t[:, 1:2],
                     in1=a_t[:], op0=ALU.mult, op1=ALU.add)
        wo = C.sb([128, 8, 1024], BF16, p1); wo_b = Buf()
        stage = C.sb([128, 8, 512], F32, p1); stage_b = Buf()
        load_w_bf16(C, wo, wo_b, wout, 1024, stage, stage_b)
        pss = Rot([(C.ps(st=p1), Buf()) for _ in range(4)])
        for blk in range(NBC):
            sl = slice(blk * 512, (blk + 1) * 512)
            for oc in range(8):
                pst, psb = pss.next()
                for kc in range(8):
                    C.op("pe", "matmul", [wo_b, y_b[blk]], [psb], pst[:], lhsT=wo[:, kc, oc * 128:(oc + 1) * 128],
                         rhs=y[:, kc, sl], start=(kc == 0), stop=(kc == 7))
                C.op("dve", "tensor_tensor", [psb, h_b[oc][blk]], [h_b[oc][blk]], out=h[:, oc, sl], in0=h[:, oc, sl],
                     in1=pst[:], op=ALU.add)
        C.S.emit(barrier=True)

    hn = C.sb([128, 8, NT], BF16); hn_b = [Buf() for _ in range(NBC)]
    if kind == "moe":
        gTs = C.sb([8, NT], F32); gTs_b = Buf()
    with ExitStack() as p2:
        sq = C.sb([128, 8, 512], BF16, p2); sq_b = Buf()
        rstd = C.sb([128, 512], F32, p2); rstd_b = Buf()
        pn = C.ps(st=p2); pn_b = Buf()
        if kind == "moe":
            hn32 = C.sb([128, 8, 512], F32, p2); hn32_b = Buf()
            r32 = C.sb([128, 8, 8], F32, p2); r32_b = Buf()
            C.dma(r32[:], router, [], [r32_b])
            ident, id_b = K["ident"]
            plg = Rot([(C.ps(st=p2), Buf()) for _ in range(2)])
            pgt = Rot([(C.ps(st=p2), Buf()) for _ in range(2)])
            sm = Rot([(C.sb([128, 64], F32, p2), Buf()) for _ in range(2)])
        for blk in range(NBC):
            sl = slice(blk * 512, (blk + 1) * 512)
            allh = [h_b[c][blk] for c in range(8)]
            hb = h[:, :, sl]
            ones_bf, ob = M["ones_bf"]
            C.op("act", "activation", allh, [sq_b], out=sq[:], in_=hb, func=AF.Square)
            for c in range(8):
                C.op("pe", "matmul", [ob, sq_b], [pn_b], pn[:], lhsT=ones_bf[:], rhs=sq[:, c, :], start=(c == 0), stop=(c == 7))
            C.op("act", "activation", [pn_b, cb], [rstd_b], out=rstd[:], in_=pn[:], func=AF.Ln, bias=cols[:, 0:1], scale=1.0 / 1024)
            C.op("act", "activation", [rstd_b], [rstd_b], out=rstd[:], in_=rstd[:], func=AF.Exp, scale=-0.5)
            for c in range(8):
                C.op("dve", "scalar_tensor_tensor", [h_b[c][blk], g_b, rstd_b], [hn_b[blk]], out=hn[:, c, sl], in0=h[:, c, sl],
                     scalar=g_t[:, c:c + 1], in1=rstd[:], op0=ALU.mult, op1=ALU.mult)
                if kind == "moe":
                    C.op("dve", "scalar_tensor_tensor", [h_b[c][blk], g_b, rstd_b], [hn32_b], out=hn32[:, c, :], in0=h[:, c, sl],
                         scalar=g_t[:, c:c + 1], in1=rstd[:], op0=ALU.mult, op1=ALU.mult)
            if kind == "moe":
                for sub in range(4):
                    lg, lg_b = plg.next()
                    for kc in range(8):
                        C.op("pe", "matmul", [hn32_b, r32_b], [lg_b], lg[:, 0:8], lhsT=hn32[:, kc, sub * 128:(sub + 1) * 128],
                             rhs=r32[:, kc, :], start=(kc == 0), stop=(kc == 7))
                    s, s_b = sm.next()
                    C.op("dve", "tensor_copy", [lg_b], [s_b], out=s[:, 0:8], in_=lg[:, 0:8])
                    C.op("dve", "reduce_max", [s_b], [s_b], out=s[:, 40:41], in_=s[:, 0:8], axis=AX.X)
                    C.op("dve", "tensor_scalar", [s_b], [s_b], out=s[:, 8:16], in0=s[:, 0:8], scalar1=s[:, 40:41], scalar2=None,
                         op0=ALU.is_equal)
                    C.op("dve", "scalar_tensor_tensor", [s_b], [s_b], out=s[:, 16:24], in0=s[:, 8:16], scalar=-1e30, in1=s[:, 0:8],
                         op0=ALU.mult, op1=ALU.add)
                    C.op("dve", "reduce_max", [s_b], [s_b], out=s[:, 41:42], in_=s[:, 16:24], axis=AX.X)
                    C.op("dve", "tensor_scalar", [s_b], [s_b], out=s[:, 24:32], in0=s[:, 16:24], scalar1=s[:, 41:42], scalar2=None,
                         op0=ALU.is_equal)
                    C.op("dve", "tensor_tensor", [s_b], [s_b], out=s[:, 42:43], in0=s[:, 41:42], in1=s[:, 40:41], op=ALU.subtract)
                    C.op("act", "activation", [s_b], [s_b], out=s[:, 44:45], in_=s[:, 42:43], func=AF.Sigmoid)
                    C.op("act", "activation", [s_b], [s_b], out=s[:, 43:44], in_=s[:, 42:43], func=AF.Sigmoid, scale=-1.0)
                    C.op("dve", "tensor_scalar", [s_b], [s_b], out=s[:, 32:40], in0=s[:, 8:16], scalar1=s[:, 43:44], scalar2=None,
                         op0=ALU.mult)
                    C.op("dve", "scalar_tensor_tensor", [s_b], [s_b], out=s[:, 32:40], in0=s[:, 24:32], scalar=s[:, 44:45],
                         in1=s[:, 32:40], op0=ALU.mult, op1=ALU.add)
                    gt, gt_b = pgt.next()
                    C.op("pe", "matmul", [s_b, id_b], [gt_b], gt[0:8, 0:128], lhsT=s[:, 32:40], rhs=ident[:], start=True, stop=True)
                    c0 = blk * 512 + sub * 128
                    C.op("act", "copy", [gt_b], [gTs_b], out=gTs[:, c0:c0 + 128], in_=gt[0:8, 0:128])
        C.S.emit(barrier=True)

    with ExitStack() as p3:
        st1 = [C.sb([128, 8, 256], F32, p3) for _ in range(2)]
        st3 = [C.sb([128, 8, 256], F32, p3) for _ in range(2)]
        st2 = [C.sb([128, 2, 1024], F32, p3) for _ in range(2)]
        st_b = [[Buf(), Buf(), Buf()] for _ in range(2)]
        wb1 = [C.sb([128, 8, 256], BF16, p3) for _ in range(2)]
        wb3 = [C.sb([128, 8, 256], BF16, p3) for _ in range(2)]
        wb2 = [C.sb([128, 2, 1024], BF16, p3) for _ in range(2)]
        wb_b = [[Buf(), Buf(), Buf()] for _ in range(2)]
        pA = Rot([(C.ps(st=p3), Buf()) for _ in range(2)])
        pB = Rot([(C.ps(st=p3), Buf()) for _ in range(2)])
        pO = Rot([(C.ps(st=p3), Buf()) for _ in range(4)])
        slu = Rot([(C.sb([128, 512], F32, p3), Buf()) for _ in range(2)])
        b3s = Rot([(C.sb([128, 512], F32, p3), Buf()) for _ in range(2)])
        hid = Rot([(C.sb([128, 2, 512], BF16, p3), Buf()) for _ in range(2)])
        if kind == "moe":
            sel, sel_b = K["sel"]
            Gt = [C.sb([128, NBC, 512], BF16, p3) for _ in range(2)]; Gt_b = [Buf(), Buf()]

        def up(e, par, blk, G, G_b):
            sl = slice(blk * 512, (blk + 1) * 512)
            hd, hd_b = hid.next()
            for fc in range(2):
                a, a_b = pA.next()
                for kc in range(8):
                    C.op("pe", "matmul", [wb_b[par][0], hn_b[blk]], [a_b], a[:], lhsT=wb1[par][:, kc, fc * 128:(fc + 1) * 128],
                         rhs=hn[:, kc, sl], start=(kc == 0), stop=(kc == 7))
                b3, b3_b = pB.next()
                for kc in range(8):
                    C.op("pe", "matmul", [wb_b[par][1], hn_b[blk]], [b3_b], b3[:], lhsT=wb3[par][:, kc, fc * 128:(fc + 1) * 128],
                         rhs=hn[:, kc, sl], start=(kc == 0), stop=(kc == 7))
                s_t, s_tb = slu.next()
                C.op("act", "activation", [a_b], [s_tb], out=s_t[:], in_=a[:], func=AF.Silu)
                c_t, c_tb = b3s.next()
                C.op("act", "copy", [b3_b], [c_tb], out=c_t[:], in_=b3[:])
                if kind == "moe":
                    C.op("pool", "tensor_tensor", [s_tb, G_b], [s_tb], out=s_t[:], in0=s_t[:], in1=G[:, blk, :], op=ALU.mult)
                C.op("pool", "tensor_tensor", [s_tb, c_tb], [hd_b], out=hd[:, fc, :], in0=s_t[:], in1=c_t[:], op=ALU.mult)
            return hd, hd_b

        def down(par, blk, hd, hd_b):
            sl = slice(blk * 512, (blk + 1) * 512)
            for dc in range(8):
                o, o_b = pO.next()
                for fc in range(2):
                    C.op("pe", "matmul", [wb_b[par][2], hd_b], [o_b], o[:], lhsT=wb2[par][:, fc, dc * 128:(dc + 1) * 128],
                         rhs=hd[:, fc, :], start=(fc == 0), stop=(fc == 1))
                C.op("dve", "tensor_tensor", [o_b, h_b[dc][blk]], [h_b[dc][blk]], out=h[:, dc, sl], in0=h[:, dc, sl],
                     in1=o[:], op=ALU.add)

        it = 0
        pending = None
        for e in range(NE):
            G, G_b = None, None
            if kind == "moe":
                G, G_b = Gt[e % 2], Gt_b[e % 2]
                for blk in range(NBC):
                    pG, pG_b = pO.next()
                    C.op("pe", "matmul", [sel_b, gTs_b], [pG_b], pG[:], lhsT=sel[0:8, e * 128:(e + 1) * 128],
                         rhs=gTs[0:8, blk * 512:(blk + 1) * 512], start=True, stop=True)
                    C.op("act", "copy", [pG_b], [G_b], out=G[:, blk, :], in_=pG[:])
            for fg in range(NFG):
                par = it % 2
                it += 1
                f0 = fg * 256
                C.dma(st1[par][:], w1[e][:, :, f0:f0 + 256], [], [st_b[par][0]])
                C.dma(st3[par][:], w3[e][:, :, f0:f0 + 256], [], [st_b[par][1]])
                C.dma(st2[par][:], w2[e][:, 2 * fg:2 * fg + 2, :], [], [st_b[par][2]])
                C.op("pool", "tensor_copy", [st_b[par][0]], [wb_b[par][0]], out=wb1[par][:], in_=st1[par][:])
                C.op("pool", "tensor_copy", [st_b[par][1]], [wb_b[par][1]], out=wb3[par][:], in_=st3[par][:])
                C.op("pool", "tensor_copy", [st_b[par][2]], [wb_b[par][2]], out=wb2[par][:], in_=st2[par][:])
                for blk in range(NBC):
                    hd, hd_b = up(e, par, blk, G, G_b)
                    if pending is not None:
                        down(*pending)
                    pending = (par, blk, hd, hd_b)
        down(*pending)
        C.S.emit(barrier=True)

    with ExitStack() as p4:
        if final:
            sq = C.sb([128, 8, 512], BF16, p4); sq_b = Buf()
            rstd = C.sb([128, 512], F32, p4); rstd_b = Buf()
            pn = C.ps(st=p4); pn_b = Buf()
            ob32 = Rot([(C.sb([128, 8, 512], F32, p4), Buf()) for _ in range(2)])
            ones_bf, ob = M["ones_bf"]
        for blk in range(NBC):
            sl = slice(blk * 512, (blk + 1) * 512)
            allh = [h_b[c][blk] for c in range(8)]
            if final:
                C.op("act", "activation", allh, [sq_b], out=sq[:], in_=h[:, :, sl], func=AF.Square)
                for c in range(8):
                    C.op("pe", "matmul", [ob, sq_b], [pn_b], pn[:], lhsT=ones_bf[:], rhs=sq[:, c, :], start=(c == 0), stop=(c == 7))
                C.op("act", "activation", [pn_b, cb], [rstd_b], out=rstd[:], in_=pn[:], func=AF.Ln, bias=cols[:, 0:1], scale=1.0 / 1024)
                C.op("act", "activation", [rstd_b], [rstd_b], out=rstd[:], in_=rstd[:], func=AF.Exp, scale=-0.5)
                o32, o32_b = ob32.next()
                for c in range(8):
                    C.op("dve", "scalar_tensor_tensor", allh + [gf_b, rstd_b], [o32_b], out=o32[:, c, :], in0=h[:, c, sl],
                         scalar=gf_t[:, c:c + 1], in1=rstd[:], op0=ALU.mult, op1=ALU.mult)
                for (dst_, lo_, hi_) in outT:
                    C.dma(dst_[:, :, sl], o32[:, lo_:hi_, :], [o32_b], out_wr)
            else:
                for (dst_, lo_, hi_) in outT:
                    C.dma(dst_[:, :, sl], h[:, lo_:hi_, sl], allh, out_wr)
    C.S.emit(barrier=True)
    C.st.close()
    C.st = old_st


def chan_inputs(inp, layer, hT_tok, yT_tok, final, pre="", perm=False):
    j = layer // 2
    d = {"gffn": np.ascontiguousarray(inp["norm_ffn"][layer].reshape(8, 128).T)}
    if layer % 2 == 0:
        d.update(wout=inp["ev_w_out"][j], w1=inp["ev_ffn_w1"][j], w3=inp["ev_ffn_w3"][j], w2=inp["ev_ffn_w2"][j])
    else:
        wo = inp["od_w_out"][j]
        if perm:
            wo = np.concatenate([wo[0:256], wo[512:768], wo[256:512], wo[768:1024]], axis=0)
        d.update(wout=np.ascontiguousarray(wo), router=inp["od_router"][j], ew1=inp["od_exp_w1"][j], ew3=inp["od_exp_w3"][j],
                 ew2=inp["od_exp_w2"][j])
    if final:
        d["gfin"] = np.ascontiguousarray(inp["norm_final"].reshape(8, 128).T)
    d = {pre + k: v for k, v in d.items()}
    if hT_tok is not None:
        d["hT"] = np.ascontiguousarray(hT_tok)
        d["yT"] = np.ascontiguousarray(yT_tok)
        if layer % 2 == 1:
            cm = const_inputs()
            d.update(c_ident=cm["c_ident"], c_sel=cm["c_sel"])
    return d


def build_mix_odd():
    C = Ctx()
    hT = C.din("hT", [1024, S_LEN]).rearrange("(c p) t -> p c t", p=128)
    yT = C.dout("yT", [512, S_LEN], BF16)
    mix_odd_body(C, "", lambda blk: [(hT[:, :, blk * 512:(blk + 1) * 512], 0, 8)], lambda r0, r1: yT[r0:r1], [], [])
    return C.close()


def mix_odd_body(C, pre, hblk, yrow, h_rd, y_wr):
    old_st = C.st
    C.st = ExitStack()
    gmix = C.din(pre + "gmix", [128, 8])
    w = C.din(pre + "w", [1024, 1552]).rearrange("(c p) n -> p c n", p=128)
    gw2 = C.din(pre + "gw2", [16, 256])
    gvec = C.din(pre + "gvec", [128, 2])
    hnorm = C.din(pre + "hnorm", [128, 4])

    K = load_consts(C, ("ident", "bdtri"))
    M = make_misc(C)
    cols, cb = M["cols"]
    ones_bf, ob = M["ones_bf"]
    ident, id_b = K["ident"]
    bdtri, bd_b = K["bdtri"]

    g_t = C.sb([128, 8], F32); g_b = Buf()
    C.dma(g_t[:], gmix, [], [g_b])
    gv_t = C.sb([128, 2], F32); gv_b = Buf()
    C.dma(gv_t[:], gvec, [], [gv_b])
    hnm = C.sb([128, 4], F32); hnm_b = Buf()
    C.dma(hnm[:], hnorm, [], [hnm_b])
    ngv = C.sb([128, 2], F32); ngv_b = Buf()
    C.op("dve", "tensor_scalar", [gv_b], [ngv_b], out=ngv[:], in0=gv_t[:], scalar1=-1.0, scalar2=None, op0=ALU.mult)
    id_bf = C.sb([128, 128], BF16); idb_b = Buf()
    C.op("dve", "tensor_copy", [id_b], [idb_b], out=id_bf[:], in_=ident[:])

    qd = C.sb([128, 2, S_LEN], BF16); qd_b = [Buf() for _ in range(NB)]
    kiT = C.sb([128, 2, S_LEN], BF16); ki_b = [Buf() for _ in range(NB)]
    kitok = C.sb([128, 32, 2, 128], BF16); kt_b = [Buf() for _ in range(32)]
    vtok = C.sb([128, 32, 512], BF16); vt_b = [Buf() for _ in range(32)]
    gS = C.sb([128, 4, S_LEN], BF16); gs_b = [Buf() for _ in range(NB)]
    dec = C.sb([128, 2, 64], F32); dec_b = Buf()

    with ExitStack() as p1:
        Wb = C.sb([128, 8, 1552], BF16, p1); Wb_b = Buf()
        hb = [C.sb([128, 8, 512], F32, p1) for _ in range(2)]; hb_b = [Buf(), Buf()]
        load_w_bf16(C, Wb, Wb_b, w, 1552, hb[1], hb_b[1])
        gw2f = C.sb([16, 256], F32, p1); gw2f_b = Buf()
        C.dma(gw2f[:], gw2, [], [gw2f_b])
        gw2b = C.sb([16, 256], BF16, p1); gw2b_b = Buf()
        C.op("dve", "tensor_copy", [gw2f_b], [gw2b_b], out=gw2b[:], in_=gw2f[:])
        mask01 = C.sb([128, 512], F32, p1); mk_b = Buf()
        C.op("pool", "memset", [], [mk_b], mask01[:], 1.0)
        C.op("pool", "memset", [mk_b], [mk_b], mask01[:].rearrange("p (c j) -> p c j", j=64)[:, :, 0:1], 0.0)
        sq = C.sb([128, 8, 512], BF16, p1); sq_b = Buf()
        rstd = C.sb([128, 512], F32, p1); rstd_b = Buf()
        hn = C.sb([128, 8, 512], BF16, p1); hn_b = Buf()
        lrb = C.sb([16, 512], BF16, p1); lrb_b = Buf()
        tE = Rot([(C.sb([128, 512], F32, p1), Buf()) for _ in range(1)])
        tB = Rot([(C.sb([128, 512], F32, p1), Buf()) for _ in range(1)])
        tQ = Rot([(C.sb([128, 512], F32, p1), Buf()) for _ in range(2)])
        tK = Rot([(C.sb([128, 512], F32, p1), Buf()) for _ in range(1)])
        pss = Rot([(C.ps(st=p1), Buf()) for _ in range(8)])
        for blk in range(NB):
            t0 = blk * 512
            sl = slice(t0, t0 + 512)
            h_t, h_b = hb[blk % 2], hb_b[blk % 2]
            for (src_, lo_, hi_) in hblk(blk):
                C.dma(h_t[:, lo_:hi_, :], src_, h_rd, [h_b])
            pst, psb = pss.next()
            norm_block(C, M, h_t, h_b, g_t, g_b, hn, hn_b, sq, sq_b, rstd, rstd_b, pst, psb)

            def proj(c0, n, pst, psb):
                for kc in range(8):
                    C.op("pe", "matmul", [Wb_b, hn_b], [psb], pst[0:n, :], lhsT=Wb[:, kc, c0:c0 + n], rhs=hn[:, kc, :],
                         start=(kc == 0), stop=(kc == 7))
            pst, psb = pss.next()
            proj(1536, 16, pst, psb)
            C.op("act", "copy", [psb], [lrb_b], out=lrb[:], in_=pst[0:16, :])
            for c in range(4):
                pst, psb = pss.next()
                proj(1024 + c * 128, 128, pst, psb)
                C.op("act", "activation", [psb], [gs_b[blk]], out=gS[:, c, sl], in_=pst[:], func=AF.Silu)
            for hl in range(2):
                pl, pl_b = pss.next()
                C.op("pe", "matmul", [gw2b_b, lrb_b], [pl_b], pl[:], lhsT=gw2b[:, hl * 128:(hl + 1) * 128], rhs=lrb[:],
                     start=True, stop=True)
                e_t, e_b = tE.next()
                C.op("act", "activation", [pl_b, ngv_b], [e_b], out=e_t[:], in_=pl[:], func=AF.Exp, scale=-1.0, bias=ngv[:, hl:hl + 1])
                C.op("act", "activation", [e_b, cb], [e_b], out=e_t[:], in_=e_t[:], func=AF.Ln, bias=cols[:, 1:2])
                B_t, B_b = tB.next()
                C.op("dve", "tensor_tensor_scan", [mk_b, e_b], [B_b], out=B_t[:], data0=mask01[:], data1=e_t[:], initial=0.0,
                     op0=ALU.mult, op1=ALU.add)
                q_t, q_b_ = tQ.next()
                k_t, k_b_ = tK.next()
                C.op("act", "activation", [B_b], [q_b_], out=q_t[:], in_=B_t[:], func=AF.Exp, scale=-1.0 / 16)
                C.op("act", "activation", [B_b], [k_b_], out=k_t[:], in_=B_t[:], func=AF.Exp, scale=1.0 / 16)
                C.op("pool", "tensor_copy", [q_b_], [dec_b], out=dec[:, hl, blk * 8:(blk + 1) * 8],
                     in_=q_t[:].rearrange("p (c j) -> p c j", j=64)[:, :, 63])
                pq, pq_b = pss.next()
                proj(hl * 128, 128, pq, pq_b)
                C.op("dve", "scalar_tensor_tensor", [pq_b, q_b_], [qd_b[blk]], out=qd[:, hl, sl], in0=pq[:], scalar=128 ** -0.5,
                     in1=q_t[:], op0=ALU.mult, op1=ALU.mult)
                pk, pk_b = pss.next()
                proj(256 + hl * 128, 128, pk, pk_b)
                C.op("dve", "tensor_tensor", [pk_b, k_b_], [ki_b[blk]], out=kiT[:, hl, sl], in0=pk[:], in1=k_t[:], op=ALU.mult)
                pt, pt_b = pss.next()
                for sub in range(4):
                    C.op("pe", "matmul", [ki_b[blk], idb_b], [pt_b], pt[:, sub * 128:(sub + 1) * 128],
                         lhsT=kiT[:, hl, t0 + sub * 128:t0 + (sub + 1) * 128], rhs=id_bf[:], start=True, stop=True)
                C.op("act", "copy", [pt_b], [kt_b[blk * 4 + s_] for s_ in range(4)], out=kitok[:, blk * 4:(blk + 1) * 4, hl, :],
                     in_=pt[:].rearrange("p (s d) -> p s d", d=128))
            for sub in range(4):
                pv, pv_b = pss.next()
                for kc in range(8):
                    C.op("pe", "matmul", [Wb_b, hn_b], [pv_b], pv[:], lhsT=hn[:, kc, sub * 128:(sub + 1) * 128],
                         rhs=Wb[:, kc, 512:1024], start=(kc == 0), stop=(kc == 7))
                tb = blk * 4 + sub
                C.op("dve" if sub % 2 else "act", "tensor_copy" if sub % 2 else "copy", [pv_b], [vt_b[tb]],
                     out=vtok[:, tb, :], in_=pv[:])
        C.S.emit(barrier=True)

    with ExitStack() as p2:
        S32 = [C.sb([128, 256], F32, p2) for _ in range(2)]; S32_b = [Buf(), Buf()]
        Sbf = [[C.sb([128, 256], BF16, p2) for _ in range(2)] for _ in range(2)]; Sbf_b = [[Buf(), Buf()] for _ in range(2)]
        Tt = [C.sb([128, 256], F32, p2) for _ in range(2)]; Tt_b = [Buf(), Buf()]
        for hl in range(2):
            C.op("pool", "memset", [], [S32_b[hl]], S32[hl][:], 0.0)
            C.op("pool", "memset", [], [Sbf_b[hl][0]], Sbf[hl][0][:], 0.0)
        pSc = Rot([(C.ps(st=p2), Buf()) for _ in range(2)])
        pOo = [[(C.ps(st=p2), Buf()) for _ in range(2)] for _ in range(2)]
        pP = (C.ps(st=p2), Buf())
        pN = (C.ps(st=p2), Buf())
        scm = Rot([(C.sb([128, 128], BF16, p2), Buf()) for _ in range(3)])
        sq2 = C.sb([128, 2, 512], BF16, p2); sq2_b = Buf()
        rs2 = C.sb([128, 512], F32, p2); rs2_b = Buf()
        ytmp = Rot([(C.sb([128, 512], F32, p2), Buf()) for _ in range(2)])
        yo = Rot([(C.sb([128, 512], BF16, p2), Buf()) for _ in range(2)])
        cur = [0, 0]
        for tb in range(32):
            blk = tb // 4
            t0 = tb * 128
            col = (tb % 4) * 128
            for hl in range(2):
                sc, sc_b = pSc.next()
                C.op("pe", "matmul", [ki_b[blk], qd_b[blk]], [sc_b], sc[:, 0:128], lhsT=kiT[:, hl, t0:t0 + 128],
                     rhs=qd[:, hl, t0:t0 + 128], start=True, stop=True)
                sm_t, sm_b = scm.next()
                C.op("dve", "tensor_tensor", [sc_b, bd_b], [sm_b], out=sm_t[:], in0=sc[:, 0:128], in1=bdtri[:], op=ALU.mult)
                for eh in range(2):
                    o, o_b = pOo[hl][eh]
                    C.op("pe", "matmul", [vt_b[tb], sm_b], [o_b], o[:, col:col + 128],
                         lhsT=vtok[:, tb, hl * 256 + eh * 128:hl * 256 + (eh + 1) * 128], rhs=sm_t[:], start=True, stop=False)
                for half in range(2):
                    n = tb * 2 + half
                    pb = 64 * half
                    c_ = cur[hl]
                    sb_t, sb_b = Sbf[hl][c_], Sbf_b[hl][c_]
                    for eh in range(2):
                        o, o_b = pOo[hl][eh]
                        C.op("pe", "matmul", [sb_b, qd_b[blk]], [o_b], o[:, col + pb:col + pb + 64],
                             lhsT=sb_t[:, eh * 128:(eh + 1) * 128], rhs=qd[:, hl, t0 + pb:t0 + pb + 64], start=False, stop=(half == 1))
                    P, P_b = pP
                    C.op("pe", "matmul", [kt_b[tb], vt_b[tb]], [P_b], P[:, 0:256], lhsT=kitok[pb:pb + 64, tb, hl, :],
                         rhs=vtok[pb:pb + 64, tb, hl * 256:(hl + 1) * 256], start=True, stop=True)
                    C.op("dve", "tensor_tensor", [P_b, S32_b[hl]], [Tt_b[hl]], out=Tt[hl][:], in0=S32[hl][:], in1=P[:, 0:256], op=ALU.add)
                    nx = 1 - c_
                    C.op("act", "mul", [Tt_b[hl], dec_b], [S32_b[hl]], out=S32[hl][:], in_=Tt[hl][:], mul=dec[:, hl, n:n + 1])
                    C.op("pool", "tensor_scalar", [Tt_b[hl], dec_b], [Sbf_b[hl][nx]], out=Sbf[hl][nx][:], in0=Tt[hl][:],
                         scalar1=dec[:, hl, n:n + 1], scalar2=None, op0=ALU.mult)
                    cur[hl] = nx
                if tb % 4 == 3:
                    sl = slice(blk * 512, (blk + 1) * 512)
                    for eh in range(2):
                        o, o_b = pOo[hl][eh]
                        C.op("act", "activation", [o_b], [sq2_b], out=sq2[:, eh, :], in_=o[:], func=AF.Square)
                    pn, pn_b = pN
                    for eh in range(2):
                        C.op("pe", "matmul", [ob, sq2_b], [pn_b], pn[:], lhsT=ones_bf[:], rhs=sq2[:, eh, :], start=(eh == 0), stop=(eh == 1))
                    C.op("act", "activation", [pn_b, cb], [rs2_b], out=rs2[:], in_=pn[:], func=AF.Ln, bias=cols[:, 0:1], scale=1.0 / 256)
                    C.op("act", "activation", [rs2_b], [rs2_b], out=rs2[:], in_=rs2[:], func=AF.Exp, scale=-0.5)
                    for eh in range(2):
                        o, o_b = pOo[hl][eh]
                        c = hl * 2 + eh
                        yt, yt_b = ytmp.next()
                        C.op("dve", "scalar_tensor_tensor", [o_b, hnm_b, rs2_b], [yt_b], out=yt[:], in0=o[:], scalar=hnm[:, c:c + 1],
                             in1=rs2[:], op0=ALU.mult, op1=ALU.mult)
                        y_t, y_b = yo.next()
                        C.op("pool", "tensor_tensor", [yt_b, gs_b[blk]], [y_b], out=y_t[:], in0=yt[:], in1=gS[:, c, sl], op=ALU.mult)
                        C.dma(yrow(c * 128, (c + 1) * 128)[:, sl], y_t[:], [y_b], y_wr)
        C.S.emit(barrier=True)
    C.S.emit(barrier=True)
    C.st.close()
    C.st = old_st


def mix_odd_inputs(inp, j, layer, b, g, hT_full, pre=""):
    W = inp["od_w_in"][j]
    w = np.concatenate([W[:, g * 256:(g + 1) * 256], W[:, 512 + g * 256:512 + (g + 1) * 256],
                        W[:, 1024 + g * 512:1024 + (g + 1) * 512], W[:, 2048 + g * 512:2048 + (g + 1) * 512],
                        W[:, 3072:3088]], axis=1)
    d = {
        "gmix": np.ascontiguousarray(inp["norm_mix"][layer].reshape(8, 128).T),
        "w": np.ascontiguousarray(w),
        "gw2": np.ascontiguousarray(inp["od_gate_w2"][j][:, g * 256:(g + 1) * 256]),
        "gvec": np.ascontiguousarray(inp["od_gate_b"][j][g * 256:(g + 1) * 256].reshape(2, 128).T),
        "hnorm": np.ascontiguousarray(inp["od_head_norm"][j][g * 512:(g + 1) * 512].reshape(4, 128).T),
    }
    d = {pre + k: v for k, v in d.items()}
    if hT_full is not None:
        d["hT"] = np.ascontiguousarray(hT_full)
        cm = const_inputs()
        d.update(c_ident=cm["c_ident"], c_bdtri=cm["c_bdtri"])
    return d


PAIRS = [[0, 1], [2, 3], [4, 5], [6, 7]]


def _gather(C, src, src_b, dst, dst_b):
    e = C.nc.gpsimd
    o = C.S.dma(lambda a=src, g=dst: e.collective_compute("AllGather", ALU.bypass, replica_groups=PAIRS, ins=[a], outs=[g]),
                [src_b], [dst_b], eng="pool")
    o.cinc = 1
    return o


def build_fused(upto=8):
    C = Ctx()
    nc = C.nc
    x_full = C.din("x_full", [1024, 4096]).rearrange("(c p) t -> p c t", p=128)
    x_own = C.din("x_own", [1024, 2048]).rearrange("(c p) t -> p c t", p=128)
    rsel = C.din("rsel", [128, 2])
    outT = C.dout("outT", [1024, 2048]).rearrange("(c p) t -> p c t", p=128)
    hprev, hprev_b = [(x_own, 0, 8)], []
    hx_all, hx_all_b = None, None
    stage = 0
    for L in range(4):
        pre = "L%d_" % L
        even = (L % 2 == 0)
        final = (L == 3)
        y_own = [nc.dram_tensor("y_own%d_%d" % (L, q), [256, 4096], BF16, kind="Internal").ap() for q in range(2)]
        y_own_b = [Buf(), Buf()]
        yrow = lambda r0, r1, y_own=y_own: y_own[r0 // 256][r0 % 256:(r1 - 1) % 256 + 1]
        if L == 0:
            hblk = lambda blk: [(x_full[:, :, blk * 512:(blk + 1) * 512], 0, 8)]
            h_rd = []
        else:
            hv = [[hx_all[q][r * 256:(r + 1) * 256, :].rearrange("(c p) t -> p c t", p=128) for q in range(4)] for r in range(2)]
            hblk = lambda blk, hv=hv: [(hv[blk // 4][q][:, :, (blk % 4) * 512:(blk % 4 + 1) * 512], 2 * q, 2 * q + 2) for q in range(4)]
            h_rd = hx_all_b
        if even:
            mix_even_body(C, pre, hblk, yrow, h_rd, y_own_b)
        else:
            mix_odd_body(C, pre, hblk, yrow, h_rd, y_own_b)
        stage += 1
        if stage >= upto:
            dbg = C.dout("dbg_y", [512, 4096], BF16)
            for q in range(2):
                C.dma(dbg[q * 256:(q + 1) * 256], y_own[q], y_own_b, [])
            break
        y_all = [nc.dram_tensor("y_all%d_%d" % (L, q), [512, 4096], BF16, kind="Internal").ap() for q in range(2)]
        y_all_b = [Buf(), Buf()]
        for q in range(2):
            _gather(C, y_own[q], y_own_b[q], y_all[q], y_all_b[q])
        if final:
            dst, dst_b = [(outT, 0, 8)], []
        else:
            hx_own = [nc.dram_tensor("hx_own%d_%d" % (L, q), [256, 2048], F32, kind="Internal").ap() for q in range(4)]
            hx_own_b = [Buf() for _ in range(4)]
            dst = [(hx_own[q].rearrange("(c p) t -> p c t", p=128), 2 * q, 2 * q + 2) for q in range(4)]
            dst_b = hx_own_b
        chan_body(C, pre, "ffn" if even else "moe", final, hprev, hprev_b, None,
                  [y_all[q].rearrange("(c p) t -> p c t", p=128) for q in range(2)], rsel, y_all_b, dst, dst_b)
        stage += 1
        if stage >= upto and not final:
            dbg = C.dout("dbg_h", [1024, 2048])
            for q in range(4):
                C.dma(dbg[q * 256:(q + 1) * 256], hx_own[q], hx_own_b, [])
            break
        if not final:
            hx_all = [nc.dram_tensor("hx_all%d_%d" % (L, q), [512, 2048], F32, kind="Internal").ap() for q in range(4)]
            hx_all_b = [Buf() for _ in range(4)]
            for q in range(4):
                _gather(C, hx_own[q], hx_own_b[q], hx_all[q], hx_all_b[q])
            hprev, hprev_b = dst, dst_b
    return C.close()


def fused_inputs(inp, c, upto=8):
    b, r = c // 2, c % 2
    xT = np.ascontiguousarray(inp["x"][b].T)
    m = {"x_full": xT, "x_own": np.ascontiguousarray(xT[:, r * 2048:(r + 1) * 2048])}
    rs = np.zeros((128, 2), np.float32); rs[:, r] = 1.0
    m["rsel"] = rs
    stage = 0
    for L in range(4):
        pre = "L%d_" % L
        j = L // 2
        if L % 2 == 0:
            m.update(mix_even_inputs(inp, j, L, b, r, None, pre))
        else:
            m.update(mix_odd_inputs(inp, j, L, b, r, None, pre))
        stage += 1
        if stage >= upto:
            break
        m.update(chan_inputs(inp, L, None, None, L == 3, pre, perm=True))
        stage += 1
        if stage >= upto:
            break
    m.update(const_inputs())
    return m


def kernel(**inp):
    inp = {k: np.asarray(v) for k, v in inp.items()}
    nc = build_fused()
    maps = [fused_inputs(inp, c) for c in range(8)]
    res = run_bass_kernel_spmd(nc, maps, core_ids=list(range(8)))
    out = np.empty((4, 4096, 1024), np.float32)
    for c in range(8):
        b, r = c // 2, c % 2
        out[b, r * 2048:(r + 1) * 2048, :] = res.results[c]["outT"].T
    return out
```
